# Optimizing a Trainium2 kernel written in Bass

```python
import jax, jax.numpy as jnp
from jax import lax
import numpy as np

D_MODEL = 4096
BATCH = 1
SEQ = 8192
DEPTH = 2

GRID_W = 64
CTX_LEN = 256
HEAD_DIM = 128
ATTN_HEADS = D_MODEL // (2 * HEAD_DIM)
ATTN_KV_HEADS = ATTN_HEADS // 4
GQA_GROUP = ATTN_HEADS // ATTN_KV_HEADS
ATTN_WIDTH = ATTN_HEADS * HEAD_DIM
KV_WIDTH = ATTN_KV_HEADS * HEAD_DIM
CONV_WIDTH = D_MODEL - ATTN_WIDTH
CONV_KERNEL = 31
GMLP_WIDTH = D_MODEL
GMLP_HEADS = 16
CHUNK = 128
Q_BLOCK = 128
ROPE_THETA = 10000.0
PEER_HEADS = 8
PEER_KEYS = 128
PEER_EXPERTS = PEER_KEYS * PEER_KEYS
PEER_KEY_DIM = 256
PEER_TOPK = 16
PEER_BLOCK = 128
LN_EPS = 1e-6
DEEPNORM_ALPHA = (2 * DEPTH) ** 0.25
DEEPNORM_BETA = (8 * DEPTH) ** -0.25
N_MOD = 6

kernel_name = "hybrid_attn_conv_gmlp_peer_dit"


def layer_norm(x, g, b):
    xf = x.astype(jnp.float32)
    mu = jnp.mean(xf, axis=-1, keepdims=True)
    var = jnp.mean(jnp.square(xf - mu), axis=-1, keepdims=True)
    return ((xf - mu) * lax.rsqrt(var + LN_EPS) * g + b).astype(x.dtype)


def rms_norm(x, g):
    xf = x.astype(jnp.float32)
    return (xf * lax.rsqrt(jnp.mean(jnp.square(xf), axis=-1, keepdims=True) + LN_EPS) * g).astype(x.dtype)


def modulate(x, shift, scale):
    return x * (1 + scale[:, None, :]) + shift[:, None, :]


def rope_2d(x, rows, cols):
    n_freq = HEAD_DIM // 4
    inv = ROPE_THETA ** (-jnp.arange(n_freq, dtype=jnp.float32) / n_freq)

    def rot(xa, pos):
        ang = pos.astype(jnp.float32)[:, None] * inv
        cos = jnp.cos(ang)[None, :, None, :]
        sin = jnp.sin(ang)[None, :, None, :]
        x1, x2 = jnp.split(xa.astype(jnp.float32), 2, axis=-1)
        return jnp.concatenate([x1 * cos - x2 * sin, x1 * sin + x2 * cos], axis=-1)

    x_row, x_col = jnp.split(x, 2, axis=-1)
    return jnp.concatenate([rot(x_row, rows), rot(x_col, cols)], axis=-1).astype(x.dtype)


def gqa_attention(q, k, v, kc, vc, q_gain, k_gain, rows, cols):
    B, S = q.shape[:2]
    q = rope_2d(rms_norm(q, q_gain), rows, cols)
    k = rope_2d(rms_norm(k, k_gain), rows, cols)
    k_all = jnp.concatenate([kc, k], axis=1)
    v_all = jnp.concatenate([vc, v], axis=1)
    n_blk = S // Q_BLOCK
    qb = q.reshape(B, n_blk, Q_BLOCK, ATTN_KV_HEADS, GQA_GROUP, HEAD_DIM).transpose(1, 0, 2, 3, 4, 5)
    scale = HEAD_DIM ** -0.5

    def one_block(q_blk):
        s = jnp.einsum('bqkgd,bskd->bkgqs', q_blk, k_all, preferred_element_type=jnp.float32) * scale
        p = jax.nn.softmax(s, axis=-1).astype(v_all.dtype)
        return jnp.einsum('bkgqs,bskd->bqkgd', p, v_all)

    o = lax.map(one_block, qb)
    return o.transpose(1, 0, 2, 3, 4, 5).reshape(B, S, ATTN_WIDTH)


def conformer_conv(a, dw_w, dw_b, ln_g, ln_b):
    val, gate = jnp.split(a, 2, axis=-1)
    h = val * jax.nn.sigmoid(gate)
    h = lax.conv_general_dilated(
        h, dw_w[:, None, :], window_strides=(1,),
        padding=[(CONV_KERNEL // 2, CONV_KERNEL // 2)],
        dimension_numbers=('NWC', 'WIO', 'NWC'),
        feature_group_count=CONV_WIDTH) + dw_b
    return jax.nn.silu(layer_norm(h, ln_g, ln_b))


def attn_conv_mixer(h, hc, rows, cols, w_in, q_gain, k_gain, dw_w, dw_b, conv_ln_g, conv_ln_b, w_out):
    B, S, _ = h.shape
    n_ctx = hc.shape[1]
    z = h @ w_in
    q, k, v, a = jnp.split(z, [ATTN_WIDTH, ATTN_WIDTH + KV_WIDTH, ATTN_WIDTH + 2 * KV_WIDTH], axis=-1)
    kvc = hc @ w_in[:, ATTN_WIDTH:ATTN_WIDTH + 2 * KV_WIDTH]
    kc, vc = jnp.split(kvc.reshape(B, n_ctx, 2 * ATTN_KV_HEADS, HEAD_DIM), 2, axis=2)
    kc = rms_norm(kc, k_gain)
    o_attn = gqa_attention(
        q.reshape(B, S, ATTN_HEADS, HEAD_DIM),
        k.reshape(B, S, ATTN_KV_HEADS, HEAD_DIM),
        v.reshape(B, S, ATTN_KV_HEADS, HEAD_DIM),
        kc, vc, q_gain, k_gain, rows, cols)
    o_conv = conformer_conv(a, dw_w, dw_b, conv_ln_g, conv_ln_b)
    return jnp.concatenate([o_attn, o_conv], axis=-1) @ w_out


def chunk_gmlp_mixer(h, w_in, sgu_ln_g, sgu_ln_b, sgu_w, sgu_b, w_out):
    B, S, _ = h.shape
    u, v = jnp.split(jax.nn.gelu(h @ w_in), 2, axis=-1)
    v = layer_norm(v, sgu_ln_g, sgu_ln_b)
    v = v.reshape(B, S // CHUNK, CHUNK, GMLP_HEADS, GMLP_WIDTH // GMLP_HEADS)
    mixed = jnp.einsum('hpq,bnqhc->bnphc', sgu_w, v) + sgu_b.T[None, None, :, :, None]
    return (u * mixed.reshape(B, S, GMLP_WIDTH)) @ w_out


def peer_ffn(h, w_q, sub_k1, sub_k2, u_tab, v_tab):
    B, S, D = h.shape
    T = B * S
    t = h.reshape(T, D)
    q = (t @ w_q).reshape(T, PEER_HEADS, PEER_KEY_DIM)
    q1, q2 = jnp.split(q, 2, axis=-1)
    s1 = jnp.einsum('thd,hkd->thk', q1, sub_k1, preferred_element_type=jnp.float32)
    s2 = jnp.einsum('thd,hkd->thk', q2, sub_k2, preferred_element_type=jnp.float32)
    v1, i1 = lax.top_k(s1, PEER_TOPK)
    v2, i2 = lax.top_k(s2, PEER_TOPK)
    cand = (v1[..., :, None] + v2[..., None, :]).reshape(T, PEER_HEADS, PEER_TOPK * PEER_TOPK)
    score, ci = lax.top_k(cand, PEER_TOPK)
    expert = (jnp.take_along_axis(i1, ci // PEER_TOPK, axis=-1) * PEER_KEYS
              + jnp.take_along_axis(i2, ci % PEER_TOPK, axis=-1))
    gate = jax.nn.softmax(score, axis=-1)
    n_blk = T // PEER_BLOCK

    def one_block(args):
        tb, eb, gb = args
        act = jax.nn.gelu(jnp.einsum('thkd,td->thk', u_tab[eb], tb, preferred_element_type=jnp.float32))
        w = (gb * act).astype(tb.dtype)
        return jnp.einsum('thk,thkd->td', w, v_tab[eb])

    out = lax.map(one_block, (t.reshape(n_blk, PEER_BLOCK, D),
                              expert.reshape(n_blk, PEER_BLOCK, PEER_HEADS, PEER_TOPK),
                              gate.reshape(n_blk, PEER_BLOCK, PEER_HEADS, PEER_TOPK)))
    return out.reshape(B, S, D)


def setup_inputs(seed: int = 0) -> dict:
    key = jax.random.key(seed)
    keys = iter(jax.random.split(key, 128))
    d = D_MODEL

    def nrm(shape, scale):
        return jax.random.normal(next(keys), shape, jnp.float32) * scale

    def gain(shape):
        return 1.0 + nrm(shape, 0.01)

    def bias(shape):
        return nrm(shape, 0.01)

    inp = {
        'x': nrm((BATCH, SEQ, d), 1.0),
        'c': nrm((BATCH, d), 1.0),
        'ctx': nrm((BATCH, CTX_LEN, d), 1.0),
        'c_ctx': nrm((d,), 1.0),
    }
    for i in range(DEPTH):
        p = 'l%d_' % i
        inp[p + 'w_mod'] = nrm((d, N_MOD * d), 0.5 * d ** -0.5)
        inp[p + 'b_mod'] = bias((N_MOD * d,))
        if i % 2 == 0:
            inp[p + 'w_in'] = nrm((d, ATTN_WIDTH + 2 * KV_WIDTH + 2 * CONV_WIDTH), d ** -0.5)
            inp[p + 'q_gain'] = gain((HEAD_DIM,))
            inp[p + 'k_gain'] = gain((HEAD_DIM,))
            inp[p + 'dw_w'] = nrm((CONV_KERNEL, CONV_WIDTH), CONV_KERNEL ** -0.5)
            inp[p + 'dw_b'] = bias((CONV_WIDTH,))
            inp[p + 'conv_ln_g'] = gain((CONV_WIDTH,))
            inp[p + 'conv_ln_b'] = bias((CONV_WIDTH,))
        else:
            inp[p + 'w_in'] = nrm((d, 2 * GMLP_WIDTH), d ** -0.5)
            inp[p + 'sgu_ln_g'] = gain((GMLP_WIDTH,))
            inp[p + 'sgu_ln_b'] = bias((GMLP_WIDTH,))
            inp[p + 'sgu_w'] = nrm((GMLP_HEADS, CHUNK, CHUNK), CHUNK ** -0.5)
            inp[p + 'sgu_b'] = gain((GMLP_HEADS, CHUNK))
        inp[p + 'w_out'] = nrm((d, d), DEEPNORM_BETA * d ** -0.5)
        inp[p + 'mix_ln_g'] = gain((d,))
        inp[p + 'mix_ln_b'] = bias((d,))
        inp[p + 'peer_wq'] = nrm((d, PEER_HEADS * PEER_KEY_DIM), d ** -0.5)
        inp[p + 'peer_k1'] = nrm((PEER_HEADS, PEER_KEYS, PEER_KEY_DIM // 2), (PEER_KEY_DIM // 2) ** -0.5)
        inp[p + 'peer_k2'] = nrm((PEER_HEADS, PEER_KEYS, PEER_KEY_DIM // 2), (PEER_KEY_DIM // 2) ** -0.5)
        inp[p + 'peer_u'] = nrm((PEER_EXPERTS, d), d ** -0.5)
        inp[p + 'peer_v'] = nrm((PEER_EXPERTS, d), DEEPNORM_BETA)
        inp[p + 'ffn_ln_g'] = gain((d,))
        inp[p + 'ffn_ln_b'] = bias((d,))
    return inp


def reference(x, c, ctx, c_ctx,
              l0_w_mod, l0_b_mod, l0_w_in, l0_q_gain, l0_k_gain, l0_dw_w, l0_dw_b, l0_conv_ln_g, l0_conv_ln_b,
              l0_w_out, l0_mix_ln_g, l0_mix_ln_b, l0_peer_wq, l0_peer_k1, l0_peer_k2, l0_peer_u, l0_peer_v,
              l0_ffn_ln_g, l0_ffn_ln_b,
              l1_w_mod, l1_b_mod, l1_w_in, l1_sgu_ln_g, l1_sgu_ln_b, l1_sgu_w, l1_sgu_b,
              l1_w_out, l1_mix_ln_g, l1_mix_ln_b, l1_peer_wq, l1_peer_k1, l1_peer_k2, l1_peer_u, l1_peer_v,
              l1_ffn_ln_g, l1_ffn_ln_b):
    B, S, D = x.shape
    n_rows = S // GRID_W
    rows = jnp.repeat(jnp.arange(n_rows, dtype=jnp.int32), GRID_W)
    cols = jnp.tile(jnp.arange(GRID_W, dtype=jnp.int32), n_rows)
    layers = (
        (l0_w_mod, l0_b_mod,
         (l0_w_in, l0_q_gain, l0_k_gain, l0_dw_w, l0_dw_b, l0_conv_ln_g, l0_conv_ln_b, l0_w_out),
         l0_mix_ln_g, l0_mix_ln_b,
         (l0_peer_wq, l0_peer_k1, l0_peer_k2, l0_peer_u, l0_peer_v),
         l0_ffn_ln_g, l0_ffn_ln_b),
        (l1_w_mod, l1_b_mod,
         (l1_w_in, l1_sgu_ln_g, l1_sgu_ln_b, l1_sgu_w, l1_sgu_b, l1_w_out),
         l1_mix_ln_g, l1_mix_ln_b,
         (l1_peer_wq, l1_peer_k1, l1_peer_k2, l1_peer_u, l1_peer_v),
         l1_ffn_ln_g, l1_ffn_ln_b),
    )
    for i in range(DEPTH):
        w_mod, b_mod, mix_p, mix_g, mix_b, peer_p, ffn_g, ffn_b = layers[i]
        shift_m, scale_m, gate_m, shift_f, scale_f, gate_f = jnp.split(
            jax.nn.silu(c) @ w_mod + b_mod, N_MOD, axis=-1)
        h = modulate(x, shift_m, scale_m)
        if i % 2 == 0:
            ctx_mod = jax.nn.silu(c_ctx)[None, :] @ w_mod[:, :2 * D] + b_mod[:2 * D]
            ctx_shift, ctx_scale = jnp.split(ctx_mod, 2, axis=-1)
            hc = modulate(ctx, ctx_shift, ctx_scale)
            y = attn_conv_mixer(h, hc, rows, cols, *mix_p)
        else:
            y = chunk_gmlp_mixer(h, *mix_p)
        x = layer_norm(DEEPNORM_ALPHA * x + gate_m[:, None, :] * y, mix_g, mix_b)
        h = modulate(x, shift_f, scale_f)
        x = layer_norm(DEEPNORM_ALPHA * x + gate_f[:, None, :] * peer_ffn(h, *peer_p), ffn_g, ffn_b)
    return x
```

```python
import numpy as np
from contextlib import ExitStack
import ml_dtypes
import concourse.bass as bass
import concourse.mybir as mybir
from concourse.bass_utils import run_bass_kernel_spmd

F32 = mybir.dt.float32
F32R = mybir.dt.float32r
BF16 = mybir.dt.bfloat16
AF = mybir.ActivationFunctionType
ALU = mybir.AluOpType

NCORES = 8
T = 1024
EXT = 1056
D = 4096
NCH = 32
EPS = 1e-6
ALPHA = 4.0 ** 0.25
SAME_ENGINE_SYNC = True
SEM_ROT = 30000
NEG = -1.0e30
GELU = AF.Gelu_apprx_tanh


class Sem:
    def __init__(self, h, dma=False):
        self.h = h
        self.total = 0
        self.dma = dma


class Dep:
    __slots__ = ("w", "r", "dsem")

    def __init__(self):
        self.w = {}
        self.r = {}
        self.dsem = None


def deps(n):
    return [Dep() for _ in range(n)]


class Ctx:
    def __init__(self, nc, n_dma_sems=84):
        self.nc = nc
        self.es = ExitStack()
        self.eng = {"pe": nc.tensor, "act": nc.scalar, "dve": nc.vector, "pool": nc.gpsimd, "sp": nc.sync}
        self.esem = {}
        self.nsem = 0
        for e in self.eng:
            self.esem[e] = self._newsem(e)
        self.known = {e: {} for e in self.eng}
        self.dfree = [self._newsem("d%d" % i, dma=True) for i in range(n_dma_sems)]
        self.phase_sems = []
        self.uid = 0

    def _newsem(self, name, dma=False):
        self.nsem += 1
        h = self.es.enter_context(self.nc.semaphore("s_%s_%d" % (name, self.nsem)))
        return Sem(h, dma)

    def sbuf(self, name, shape, dt, stack=None):
        self.uid += 1
        return (stack or self.es).enter_context(self.nc.sbuf_tensor("%s_%d" % (name, self.uid), list(shape), dt))

    def psum(self, name, shape, dt=F32, stack=None):
        self.uid += 1
        return (stack or self.es).enter_context(self.nc.psum_tensor("%s_%d" % (name, self.uid), list(shape), dt))

    def dsem_for(self, dep):
        if dep.dsem is None:
            if not self.dfree:
                raise RuntimeError("out of dma sems")
            dep.dsem = self.dfree.pop(0)
            self.phase_sems.append(dep)
        return dep.dsem

    def _collect(self, reads, writes, pwrites=()):
        waits = {}

        def upd(dd):
            for s, v in dd.items():
                if waits.get(s, 0) < v:
                    waits[s] = v
        for d in reads:
            upd(d.w)
        for d in writes:
            upd(d.w)
            upd(d.r)
        for d in pwrites:
            upd(d.r)
        return waits

    def _emit_waits(self, e, waits):
        eng = self.eng[e]
        kn = self.known[e]
        own = self.esem[e]
        for s, v in waits.items():
            if s.dma:
                v = s.total
            if s is own and (e == "pe" or not SAME_ENGINE_SYNC):
                continue
            if kn.get(s, 0) >= v:
                continue
            eng.wait_ge(s.h, v)
            kn[s] = v

    def _record(self, s, tok, reads, writes, pwrites):
        for d in reads:
            if d.r.get(s, 0) < tok:
                d.r[s] = tok
        for d in writes:
            d.w = {s: tok}
            d.r = {}
        for d in pwrites:
            d.w[s] = tok

    def op(self, e, fn, reads=(), writes=(), pwrites=(), inc=True):
        self._emit_waits(e, self._collect(reads, writes, pwrites))
        ins = fn(self.eng[e])
        s = self.esem[e]
        if inc:
            if s.total >= SEM_ROT:
                s = self.esem[e] = self._newsem(e)
            s.total += 1
            ins.then_inc(s.h, 1)
            tok = s.total
        else:
            tok = s.total + 1
        self._record(s, tok, reads, writes, pwrites)
        return ins

    def dma(self, q, out, in_, reads=(), writes=(), pwrites=(), owner=None, **kw):
        self._emit_waits(q, self._collect(reads, writes, pwrites))
        if owner is None:
            owner = writes[0] if writes else (pwrites[0] if pwrites else reads[0])
        s = self.dsem_for(owner)
        ins = self.eng[q].dma_start(out=out, in_=in_, **kw)
        s.total += 16
        ins.then_inc(s.h, 16)
        self._record(s, s.total, reads, writes, pwrites)
        return ins

    def barrier(self):
        allw = {}
        for e, s in self.esem.items():
            if s.total > 0:
                allw[s] = s.total
        for d in self.phase_sems:
            if d.dsem.total > 0:
                allw[d.dsem] = d.dsem.total
        for e in self.eng:
            self._emit_waits(e, {s: v for s, v in allw.items() if s is not self.esem[e]})
        for d in self.phase_sems:
            self.dfree.append(d.dsem)
            d.dsem = None
        self.phase_sems = []

    def finish(self):
        self.barrier()
        self.es.close()


class Prog:
    def __init__(self, stage):
        self.stage = stage
        nc = bass.Bass("TRN2", target_bir_lowering=False)
        nc.dge_precook = False
        self.nc = nc
        self.c = Ctx(nc)
        self.ddeps = {}
        self.dram = {}

    def din(self, name, shape, dt):
        ap = self.nc.dram_tensor(name, list(shape), dt, kind="ExternalInput").ap()
        self.dram[name] = ap
        self.ddeps[name] = Dep()
        return ap

    def dout(self, name, shape, dt):
        ap = self.nc.dram_tensor(name, list(shape), dt, kind="ExternalOutput").ap()
        self.dram[name] = ap
        self.ddeps[name] = Dep()
        return ap

    def dint(self, name, shape, dt):
        ap = self.nc.dram_tensor(name, list(shape), dt, kind="Internal").ap()
        self.dram[name] = ap
        self.ddeps[name] = Dep()
        return ap

    def load(self, q, out, name, in_ap, dep, partial=False, **kw):
        dd = self.ddeps[name]
        if partial:
            return self.c.dma(q, out, in_ap, reads=[dd], pwrites=[dep], owner=dep, **kw)
        return self.c.dma(q, out, in_ap, reads=[dd], writes=[dep], owner=dep, **kw)

    def store(self, q, name, out_ap, in_, dep, **kw):
        dd = self.ddeps[name]
        return self.c.dma(q, out_ap, in_, reads=[dep], pwrites=[dd], owner=dep, **kw)


def cast_dma_kw():
    return dict(max_dma_last_dim=4096)


def gemm_fm(P, ph, pfx, wname, wd, n_ct, rhs, rhs_deps, nchunks, epi, nk=NCH, ct_order=None):
    c = P.c
    nslot = 3
    with ExitStack() as gs:
        ws = [c.sbuf(pfx + "_w", [128, nk, 128], BF16, gs) for _ in range(nslot)]
        wdp = deps(nslot)
        npb = len(nchunks)
        nbuf = 2 if 2 * npb <= 6 else 1
        ps = [[c.psum(pfx + "_p", [128, 512], F32, gs) for _ in range(npb)] for _ in range(nbuf)]
        psd = [deps(npb) for _ in range(nbuf)]
        order = list(range(n_ct)) if ct_order is None else ct_order
        pend = None
        for it, ct in enumerate(order):
            s = it % nslot
            P.load("pool", ws[s][:], wname, wd[ct], wdp[s], **cast_dma_kw())
            b = it % nbuf
            for j, (c0, n) in enumerate(nchunks):
                for k in range(nk):
                    c.op("pe", lambda e: e.matmul(ps[b][j][:, :n], ws[s][:, k, :], rhs[:, k, c0:c0 + n],
                                                  start=(k == 0), stop=(k == nk - 1)),
                         reads=[wdp[s], rhs_deps[k]], writes=[psd[b][j]], inc=(k == nk - 1))
                if pend is not None:
                    epi(*pend)
                pend = (ct, j, ps[b][j], psd[b][j], n)
        if pend is not None:
            epi(*pend)
        c.barrier()


def ln_phase(P, pfx, uname, gb_name, xout_name, hT=None, hT_deps=None, mod_sb=None, lidx=0, mshift=0, mscale=1):
    c = P.c
    u_d = P.dram[uname]
    x_d = P.dram[xout_name]
    with ExitStack() as ph:
        ones = c.sbuf(pfx + "ones", [128, 128], F32R, ph)
        d_ones = Dep()
        P.load("sp", ones[:], "ones_r", P.dram["ones_r"], d_ones)
        gb = c.sbuf(pfx + "gb", [128, 2, NCH], F32, ph)
        d_gb = Dep()
        P.load("sp", gb[:], gb_name, P.dram[gb_name], d_gb)
        ub = [c.sbuf(pfx + "u", [128, T], F32R, ph) for _ in range(3)]
        ubd = deps(3)
        sq = [c.sbuf(pfx + "sq", [128, T], F32R, ph) for _ in range(2)]
        sqd = deps(2)
        pss = [c.psum(pfx + "ps", [128, 512], F32, ph) for _ in range(4)]
        pssd = deps(4)
        for ch in range(NCH):
            s = ch % 3
            P.load("sp", ub[s][:], uname, u_d[ch], ubd[s])
            q = ch % 2
            c.op("act", lambda e: e.activation(out=sq[q][:], in_=ub[s][:], func=AF.Square), reads=[ubd[s]], writes=[sqd[q]])
            for half in range(2):
                sl = slice(half * 512, half * 512 + 512)
                c.op("pe", lambda e: e.matmul(pss[half][:], ones[:], ub[s][:, sl], start=(ch == 0), stop=(ch == NCH - 1)),
                     reads=[d_ones, ubd[s]], writes=[pssd[half]], inc=True)
                c.op("pe", lambda e: e.matmul(pss[2 + half][:], ones[:], sq[q][:, sl], start=(ch == 0), stop=(ch == NCH - 1)),
                     reads=[d_ones, sqd[q]], writes=[pssd[2 + half]], inc=True)
        mean = c.sbuf(pfx + "mean", [128, T], F32, ph)
        rstd = c.sbuf(pfx + "rstd", [128, T], F32, ph)
        nmr = c.sbuf(pfx + "nmr", [128, T], F32, ph)
        tmp = c.sbuf(pfx + "tmp", [128, T], F32, ph)
        d_mean, d_rstd, d_nmr, d_tmp = deps(4)
        for half in range(2):
            sl = slice(half * 512, half * 512 + 512)
            c.op("act", lambda e: e.mul(mean[:, sl], pss[half][:], 1.0 / D), reads=[pssd[half]], pwrites=[d_mean])
            c.op("dve", lambda e: e.tensor_tensor(out=tmp[:, sl], in0=mean[:, sl], in1=mean[:, sl], op=ALU.mult), reads=[d_mean], pwrites=[d_tmp])
            c.op("dve", lambda e: e.scalar_tensor_tensor(out=rstd[:, sl], in0=pss[2 + half][:], scalar=1.0 / D, in1=tmp[:, sl],
                                                         op0=ALU.mult, op1=ALU.subtract), reads=[pssd[2 + half], d_tmp], pwrites=[d_rstd])
            c.op("dve", lambda e: e.tensor_scalar(out=rstd[:, sl], in0=rstd[:, sl], scalar1=EPS, scalar2=None, op0=ALU.add), reads=[d_rstd], pwrites=[d_rstd])
            c.op("act", lambda e: e.activation(out=rstd[:, sl], in_=rstd[:, sl], func=AF.Sqrt), reads=[d_rstd], pwrites=[d_rstd])
            c.op("dve", lambda e: e.reciprocal(out=rstd[:, sl], in_=rstd[:, sl]), reads=[d_rstd], pwrites=[d_rstd])
            c.op("dve", lambda e: e.scalar_tensor_tensor(out=nmr[:, sl], in0=mean[:, sl], scalar=-1.0, in1=rstd[:, sl],
                                                         op0=ALU.mult, op1=ALU.mult), reads=[d_mean, d_rstd], pwrites=[d_nmr])
        if hT is not None:
            sc1 = c.sbuf(pfx + "sc1", [128, NCH], F32, ph)
            d_sc1 = Dep()
            c.op("dve", lambda e: e.tensor_scalar(out=sc1[:], in0=mod_sb[:, lidx, 0, mscale * NCH:(mscale + 1) * NCH], scalar1=1.0, scalar2=None, op0=ALU.add),
                 reads=[P.d_mod], writes=[d_sc1])
        xn = [c.sbuf(pfx + "xn", [128, T], F32, ph) for _ in range(2)]
        xnd = deps(2)
        for ch in range(NCH):
            s = ch % 3
            q = ch % 2
            P.load("sp", ub[s][:], uname, u_d[ch], ubd[s])
            uf = ub[s][:].bitcast(F32)
            c.op("dve", lambda e: e.tensor_tensor(out=xn[q][:], in0=uf, in1=rstd[:], op=ALU.mult), reads=[ubd[s], d_rstd], writes=[xnd[q]])
            c.op("pool", lambda e: e.tensor_tensor(out=xn[q][:], in0=xn[q][:], in1=nmr[:], op=ALU.add), reads=[xnd[q], d_nmr], writes=[xnd[q]])
            c.op("dve", lambda e: e.tensor_scalar(out=xn[q][:], in0=xn[q][:], scalar1=gb[:, 0, ch:ch + 1], scalar2=gb[:, 1, ch:ch + 1],
                                                  op0=ALU.mult, op1=ALU.add), reads=[xnd[q], d_gb], writes=[xnd[q]])
            P.store("sp", xout_name, x_d[ch], xn[q][:], xnd[q])
            if hT is not None:
                c.op("act", lambda e: e.activation(out=hT[:, ch, :], in_=xn[q][:], func=AF.Identity,
                                                   scale=sc1[:, ch:ch + 1], bias=mod_sb[:, lidx, 0, mshift * NCH + ch:mshift * NCH + ch + 1]),
                     reads=[xnd[q], d_sc1, P.d_mod], writes=[hT_deps[ch]])
        c.barrier()


def build(stage):
    P = Prog(stage)
    c = P.c
    nc = P.nc
    if stage == "mod":
        cc = P.din("cc", [128, NCH, 2], F32)
        wm = [P.din("wm%d" % l, [NCH, 128, 3072], F32) for l in range(2)]
        bm = P.din("bm", [2, 2, 3072], F32)
        mo = P.dout("modout", [2, 2, 3072], F32)
        with ExitStack() as ph:
            cs = c.sbuf("cs", [128, NCH, 2], F32, ph)
            d_cs = Dep()
            P.load("sp", cs[:], "cc", cc, d_cs)
            c.op("act", lambda e: e.activation(out=cs[:], in_=cs[:], func=AF.Silu), reads=[d_cs], writes=[d_cs])
            bs = c.sbuf("bs", [2, 2, 3072], F32, ph)
            d_bs = Dep()
            P.load("sp", bs[:], "bm", bm.rearrange("l r n -> r l n"), d_bs)
            res = c.sbuf("res", [2, 2, 3072], F32, ph)
            d_res = Dep()
            ws = [c.sbuf("wms", [128, 3072], F32, ph) for _ in range(3)]
            wsd = deps(3)
            ps = [c.psum("mps", [128, 512], F32, ph) for _ in range(6)]
            psd = deps(6)
            for l in range(2):
                for k in range(NCH):
                    s = k % 3
                    P.load("sp", ws[s][:], "wm%d" % l, wm[l][k], wsd[s])
                    for j in range(6):
                        c.op("pe", lambda e: e.matmul(ps[j][0:2, :], cs[:, k, :], ws[s][:, j * 512:(j + 1) * 512],
                                                      start=(k == 0), stop=(k == NCH - 1)),
                             reads=[d_cs, wsd[s]], writes=[psd[j]], inc=True)
                for j in range(6):
                    c.op("dve", lambda e: e.tensor_tensor(out=res[:, l, j * 512:(j + 1) * 512], in0=ps[j][0:2, :],
                                                          in1=bs[:, l, j * 512:(j + 1) * 512], op=ALU.add),
                         reads=[psd[j], d_bs], pwrites=[d_res])
            P.store("sp", "modout", mo.rearrange("l r n -> r l n"), res[:], d_res)
            c.barrier()
        c.finish()
        return nc

    FUSED = stage == "all"
    ident_r = P.din("ident_r", [128, 128], F32R)
    ones_r = P.din("ones_r", [128, 128], F32R)
    mod_sb = c.sbuf("mod_sb", [128, 2, 2, 192], F32)
    P.d_mod = Dep()
    if not FUSED:
        modv = P.din("modv", [128, 2, 2, 192], F32)
        P.load("sp", mod_sb[:], "modv", modv, P.d_mod)
    else:
        ccr = P.din("cc", [128, NCH, 2], F32)
        wmT = [P.din("wmT%d" % l, [192, 128, NCH, 128], F32R) for l in range(2)]
        bmT = P.din("bmT", [128, 2, 192], F32)
        with ExitStack() as ph:
            cs0 = c.sbuf("mcs0", [128, NCH, 2], F32, ph); d_cs0 = Dep()
            cs = c.sbuf("mcs", [128, NCH, 2], F32R, ph); d_cs = Dep()
            P.load("sp", cs0[:], "cc", ccr, d_cs0)
            c.op("act", lambda e: e.activation(out=cs[:], in_=cs0[:], func=AF.Silu), reads=[d_cs0], writes=[d_cs])
            bs = c.sbuf("mbs", [128, 2, 192], F32, ph); d_bs = Dep()
            P.load("sp", bs[:], "bmT", bmT, d_bs)
            wsl = [c.sbuf("mws", [128, NCH, 128], F32R, ph) for _ in range(4)]; wsd = deps(4)
            mps = [c.psum("mps", [128, 512], F32, ph) for _ in range(2)]; mpd = deps(2)
            nq = 0
            for l in range(2):
                for j in range(192):
                    s = nq % 4; nq += 1
                    P.load("sp" if nq % 2 else "act", wsl[s][:], "wmT%d" % l, wmT[l][j], wsd[s])
                    for k in range(NCH):
                        c.op("pe", lambda e: e.matmul(mps[l][:, 2 * j:2 * j + 2], wsl[s][:, k, :], cs[:, k, :], start=(k == 0), stop=(k == NCH - 1)),
                             reads=[wsd[s], d_cs], writes=[mpd[l]], inc=(k == NCH - 1))
                c.op("dve", lambda e: e.tensor_tensor(out=mod_sb[:, l, :, :], in0=mps[l][:, 0:384].rearrange("p (j r) -> p r j", r=2),
                                                      in1=bs[:, l, :].unsqueeze(1).to_broadcast([128, 2, 192]), op=ALU.add),
                     reads=[mpd[l], d_bs], pwrites=[P.d_mod])
            c.barrier()

    if stage in ("l0a", "all"):
        mk = P.dint if FUSED else P.dout
        xT = P.din("xT", [128, NCH, EXT], F32)
        hmask = P.din("hmask", [1, EXT], F32)
        ctxT = P.din("ctxT", [128, NCH, 256], F32)
        w_in0 = P.din("w_in0", [56, 128, NCH, 128], F32)
        gains = P.din("gains", [128, 2], F32)
        cosT = P.din("cosT", [128, T], F32)
        sinT = P.din("sinT", [128, T], F32)
        Rm = P.din("Rm", [128, 128], F32R)
        convp = P.din("convp", [128, 16, 34], F32)
        QT = mk("QT", [16, 128, T], BF16)
        KTl = mk("KTl", [4, 128, T], BF16)
        Vl = mk("Vl", [4, T, 128], BF16)
        KTc = mk("KTc", [4, 128, 256], BF16)
        Vc = mk("Vc", [4, 256, 128], BF16)
        featc = mk("featc", [16, 128, T], BF16)
        Gd = P.dint("Gd", [16, 128, EXT], F32R)
        if FUSED:
            xTa = P.din("xTa", [128, NCH, 8192], F32)
            cosA = P.din("cosA", [128, 8192], F32)
            sinA = P.din("sinA", [128, 8192], F32)
            w_v0 = P.din("w_v0", [128, NCH, 512], F32)
            KTa = P.dint("KTa", [4, 128, 8448], BF16)
            Va = P.dint("Va", [4, 128, 66, 128], BF16)
            with ExitStack() as ph:
                wk = c.sbuf("kvwk", [128, 4, NCH, 128], BF16, ph); d_wk = Dep()
                wv = c.sbuf("kvwv", [128, NCH, 512], BF16, ph); d_wv = Dep()
                for g in range(4):
                    P.load("pool", wk[:, g], "w_in0", w_in0[16 + g], d_wk, partial=True, **cast_dma_kw())
                P.load("pool", wv[:], "w_v0", w_v0, d_wv, **cast_dma_kw())
                hb = [c.sbuf("kvh", [128, NCH, 512], BF16, ph) for _ in range(2)]; hbd = [deps(NCH) for _ in range(2)]
                sc1 = c.sbuf("kvsc1", [128, 2, NCH], F32, ph); d_sc1 = Dep()
                c.op("dve", lambda e: e.tensor_scalar(out=sc1[:, 0, :], in0=mod_sb[:, 0, 0, 32:64], scalar1=1.0, scalar2=None, op0=ALU.add), reads=[P.d_mod], pwrites=[d_sc1])
                c.op("dve", lambda e: e.tensor_scalar(out=sc1[:, 1, :], in0=mod_sb[:, 0, 1, 32:64], scalar1=1.0, scalar2=None, op0=ALU.add), reads=[P.d_mod], pwrites=[d_sc1])
                xs = [c.sbuf("kvxs", [128, 512], F32, ph) for _ in range(6)]; xsd = deps(6)
                csn = [c.sbuf("kvcs", [128, 2, 512], F32, ph) for _ in range(2)]; csnd = deps(2)
                gn = c.sbuf("kvgn", [128, 2], F32, ph); d_gn = Dep()
                P.load("sp", gn[:], "gains", gains, d_gn)
                rm_sb = c.sbuf("kvrm", [128, 128], F32R, ph); d_rm = Dep()
                P.load("sp", rm_sb[:], "Rm", Rm, d_rm)
                on_sb = c.sbuf("kvones", [128, 128], F32R, ph); d_on = Dep()
                P.load("sp", on_sb[:], "ones_r", ones_r, d_on)
                sq = [c.sbuf("kvsq", [128, 512], F32R, ph) for _ in range(2)]; sqd = deps(2)
                rin = [c.sbuf("kvrin", [128, 512], F32, ph) for _ in range(2)]; rind = deps(2)
                qn = [c.sbuf("kvqn", [128, 512], F32R, ph) for _ in range(2)]; qnd = deps(2)
                t1 = [c.sbuf("kvt1", [128, 512], F32, ph) for _ in range(2)]; t1d = deps(2)
                t2 = [c.sbuf("kvt2", [128, 512], F32, ph) for _ in range(2)]; t2d = deps(2)
                ko = [c.sbuf("kvko", [128, 512], BF16, ph) for _ in range(3)]; kod = deps(3)
                vo2 = [c.sbuf("kvvo", [128, 4, 128], BF16, ph) for _ in range(3)]; vo2d = deps(3)
                pk = [c.psum("kvpk", [128, 512], F32, ph) for _ in range(4)]; pkd = deps(4)
                pss = c.psum("kvps", [128, 512], F32, ph); pssd = Dep()
                psr = c.psum("kvpr", [128, 512], F32, ph); psrd = Dep()
                pv2 = [c.psum("kvpv", [128, 512], F32, ph) for _ in range(2)]; pv2d = deps(2)
                nx = [0]; nk_ = 0; nv_ = 0

                def modulate(blk):
                    b = blk % 2
                    n = 512 if blk < 16 else 256
                    row = 0 if blk < 16 else 1
                    for ch in range(NCH):
                        s4 = nx[0] % 6; nx[0] += 1
                        src = xTa[:, ch, blk * 512:(blk + 1) * 512] if blk < 16 else ctxT[:, ch, :]
                        P.load("sp", xs[s4][:, :n], "xTa" if blk < 16 else "ctxT", src, xsd[s4])
                        if ch % 2:
                            c.op("dve", lambda e: e.tensor_scalar(out=hb[b][:, ch, :n], in0=xs[s4][:, :n], scalar1=sc1[:, row, ch:ch + 1],
                                                                  scalar2=mod_sb[:, 0, row, ch:ch + 1], op0=ALU.mult, op1=ALU.add),
                                 reads=[xsd[s4], d_sc1, P.d_mod], writes=[hbd[b][ch]])
                        else:
                            c.op("act", lambda e: e.activation(out=hb[b][:, ch, :n], in_=xs[s4][:, :n], func=AF.Identity,
                                                               scale=sc1[:, row, ch:ch + 1], bias=mod_sb[:, 0, row, ch:ch + 1]),
                                 reads=[xsd[s4], d_sc1, P.d_mod], writes=[hbd[b][ch]])
                    if blk < 16:
                        P.load("sp", csn[b][:, 0, :], "cosA", cosA[:, blk * 512:(blk + 1) * 512], csnd[b], partial=True)
                        P.load("sp", csn[b][:, 1, :], "sinA", sinA[:, blk * 512:(blk + 1) * 512], csnd[b], partial=True)

                modulate(0)
                for blk in range(17):
                    b = blk % 2
                    n = 512 if blk < 16 else 256
                    for g in range(4):
                        for k in range(NCH):
                            c.op("pe", lambda e: e.matmul(pk[g][:, :n], wk[:, g, k, :], hb[b][:, k, :n], start=(k == 0), stop=(k == NCH - 1)),
                                 reads=[d_wk, hbd[b][k]], writes=[pkd[g]], inc=(k == NCH - 1))
                    for g in range(4):
                        i = nk_ % 2; o3 = nk_ % 3; nk_ += 1
                        ps, psd = pk[g], pkd[g]
                        c.op("act", lambda e: e.activation(out=sq[i][:, :n], in_=ps[:, :n], func=AF.Square), reads=[psd], writes=[sqd[i]])
                        c.op("pe", lambda e: e.matmul(pss[:, :n], on_sb[:], sq[i][:, :n], start=True, stop=True), reads=[d_on, sqd[i]], writes=[pssd])
                        c.op("act", lambda e: e.mul(rin[i][:, :n], pss[:, :n], 1.0 / 128.0), reads=[pssd], writes=[rind[i]])
                        c.op("dve", lambda e: e.tensor_scalar(out=rin[i][:, :n], in0=rin[i][:, :n], scalar1=EPS, scalar2=None, op0=ALU.add), reads=[rind[i]], writes=[rind[i]])
                        c.op("act", lambda e: e.activation(out=rin[i][:, :n], in_=rin[i][:, :n], func=AF.Sqrt), reads=[rind[i]], writes=[rind[i]])
                        c.op("dve", lambda e: e.reciprocal(out=rin[i][:, :n], in_=rin[i][:, :n]), reads=[rind[i]], writes=[rind[i]])
                        if blk == 16:
                            c.op("dve", lambda e: e.scalar_tensor_tensor(out=ko[o3][:, :n], in0=ps[:, :n], scalar=gn[:, 1:2], in1=rin[i][:, :n],
                                                                         op0=ALU.mult, op1=ALU.mult), reads=[psd, d_gn, rind[i]], writes=[kod[o3]])
                            P.store("sp", "KTa", KTa[g][:, 0:256], ko[o3][:, :n], kod[o3])
                        else:
                            c.op("dve", lambda e: e.scalar_tensor_tensor(out=qn[i][:, :n], in0=ps[:, :n], scalar=gn[:, 1:2], in1=rin[i][:, :n],
                                                                         op0=ALU.mult, op1=ALU.mult), reads=[psd, d_gn, rind[i]], writes=[qnd[i]])
                            c.op("pe", lambda e: e.matmul(psr[:, :n], rm_sb[:], qn[i][:, :n], start=True, stop=True), reads=[d_rm, qnd[i]], writes=[psrd])
                            c.op("pool", lambda e: e.tensor_tensor(out=t1[i][:, :n], in0=qn[i][:, :n].bitcast(F32), in1=csn[b][:, 0, :], op=ALU.mult), reads=[qnd[i], csnd[b]], writes=[t1d[i]])
                            c.op("dve", lambda e: e.tensor_tensor(out=t2[i][:, :n], in0=psr[:, :n], in1=csn[b][:, 1, :], op=ALU.mult), reads=[psrd, csnd[b]], writes=[t2d[i]])
                            c.op("dve", lambda e: e.tensor_tensor(out=ko[o3][:, :n], in0=t1[i][:, :n], in1=t2[i][:, :n], op=ALU.add), reads=[t1d[i], t2d[i]], writes=[kod[o3]])
                            P.store("sp", "KTa", KTa[g][:, 256 + blk * 512:256 + (blk + 1) * 512], ko[o3][:, :n], kod[o3])
                    if blk + 1 < 17:
                        modulate(blk + 1)
                    for tt in range(n // 128):
                        pb = nv_ % 2; o3 = nv_ % 3; nv_ += 1
                        for k in range(NCH):
                            c.op("pe", lambda e: e.matmul(pv2[pb][:], hb[b][:, k, tt * 128:(tt + 1) * 128], wv[:, k, :], start=(k == 0), stop=(k == NCH - 1)),
                                 reads=[hbd[b][k], d_wv], writes=[pv2d[pb]], inc=(k == NCH - 1))
                        c.op("act", lambda e: e.copy(vo2[o3][:].rearrange("p g d -> p (g d)"), pv2[pb][:]), reads=[pv2d[pb]], writes=[vo2d[o3]])
                        st = (2 + blk * 4 + tt) if blk < 16 else tt
                        P.store("sp", "Va", Va[:, :, st, :].rearrange("g p d -> p g d"), vo2[o3][:], vo2d[o3])
                c.barrier()
        with ExitStack() as ph:
            hT = c.sbuf("hT", [128, NCH, EXT], BF16, ph)
            hTd = deps(NCH)
            hcT = c.sbuf("hcT", [128, NCH, 256], BF16, ph)
            hcTd = deps(NCH)
            sc1 = c.sbuf("sc1", [128, 2, NCH], F32, ph)
            d_sc1 = Dep()
            c.op("dve", lambda e: e.tensor_scalar(out=sc1[:, 0, :], in0=mod_sb[:, 0, 0, 32:64], scalar1=1.0, scalar2=None, op0=ALU.add),
                 reads=[P.d_mod], pwrites=[d_sc1])
            c.op("dve", lambda e: e.tensor_scalar(out=sc1[:, 1, :], in0=mod_sb[:, 0, 1, 32:64], scalar1=1.0, scalar2=None, op0=ALU.add),
                 reads=[P.d_mod], pwrites=[d_sc1])
            xs = [c.sbuf("xs", [128, EXT], F32, ph) for _ in range(3)]
            xsd = deps(3)
            cxs = [c.sbuf("cxs", [128, 256], F32, ph) for _ in range(2)]
            cxd = deps(2)
            for ch in range(NCH):
                s = ch % 3
                P.load("sp", xs[s][:], "xT", xT[:, ch, :], xsd[s])
                c.op("dve", lambda e: e.tensor_scalar(out=hT[:, ch, :], in0=xs[s][:], scalar1=sc1[:, 0, ch:ch + 1],
                                                      scalar2=mod_sb[:, 0, 0, ch:ch + 1], op0=ALU.mult, op1=ALU.add),
                     reads=[xsd[s], d_sc1, P.d_mod], writes=[hTd[ch]])
                s2 = ch % 2
                if FUSED:
                    continue
                P.load("sp", cxs[s2][:], "ctxT", ctxT[:, ch, :], cxd[s2])
                c.op("pool", lambda e: e.tensor_scalar(out=hcT[:, ch, :], in0=cxs[s2][:], scalar1=sc1[:, 1, ch:ch + 1],
                                                       scalar2=mod_sb[:, 0, 1, ch:ch + 1], op0=ALU.mult, op1=ALU.add),
                     reads=[cxd[s2], d_sc1, P.d_mod], writes=[hcTd[ch]])
            gn = c.sbuf("gn", [128, 2], F32, ph); d_gn = Dep()
            P.load("sp", gn[:], "gains", gains, d_gn)
            cs_sb = c.sbuf("cos", [128, T], F32, ph); sn_sb = c.sbuf("sin", [128, T], F32, ph); d_cs = Dep()
            P.load("sp", cs_sb[:], "cosT", cosT, d_cs, partial=True)
            P.load("sp", sn_sb[:], "sinT", sinT, d_cs, partial=True)
            rm_sb = c.sbuf("rm", [128, 128], F32R, ph); d_rm = Dep()
            P.load("sp", rm_sb[:], "Rm", Rm, d_rm)
            on_sb = c.sbuf("ones", [128, 128], F32R, ph); d_on = Dep()
            P.load("sp", on_sb[:], "ones_r", ones_r, d_on)
            hm = c.sbuf("hm", [128, EXT], F32, ph); d_hm = Dep()
            P.load("sp", hm[:], "hmask", hmask.partition_broadcast(128), d_hm)
            sq = [c.sbuf("sq", [128, 512], F32R, ph) for _ in range(2)]; sqd = deps(2)
            rin = [c.sbuf("rin", [128, 512], F32, ph) for _ in range(2)]; rind = deps(2)
            qn = [c.sbuf("qn", [128, 512], F32R, ph) for _ in range(2)]; qnd = deps(2)
            t1 = [c.sbuf("t1", [128, 512], F32, ph) for _ in range(2)]; t1d = deps(2)
            t2 = [c.sbuf("t2", [128, 512], F32, ph) for _ in range(2)]; t2d = deps(2)
            qo = [c.sbuf("qo", [128, T], BF16, ph) for _ in range(2)]; qod = deps(2)
            ps_s = [c.psum("ps_s", [128, 512], F32, ph) for _ in range(1)]; ps_sd = deps(1)
            ps_r = [c.psum("ps_r", [128, 512], F32, ph) for _ in range(1)]; ps_rd = deps(1)
            vo = [c.sbuf("vo", [128, 128], BF16, ph) for _ in range(2)]; vod = deps(2)
            valb = c.sbuf("valb", [128, EXT], F32, ph); d_valb = Dep()
            sig = c.sbuf("sig", [128, EXT], F32, ph); d_sig = Dep()
            gb = [c.sbuf("gb", [128, EXT], F32R, ph) for _ in range(2)]; gbd = deps(2)
            cnt = {"n": 0}

            def normrope(ps, psd, n, gcol, rope, dst, dstd, dsl):
                i = cnt["n"] % 2
                cnt["n"] += 1
                c.op("act", lambda e: e.activation(out=sq[i][:, :n], in_=ps[:, :n], func=AF.Square), reads=[psd], writes=[sqd[i]])
                c.op("pe", lambda e: e.matmul(ps_s[0][:, :n], on_sb[:], sq[i][:, :n], start=True, stop=True), reads=[d_on, sqd[i]], writes=[ps_sd[0]])
                c.op("act", lambda e: e.mul(rin[i][:, :n], ps_s[0][:, :n], 1.0 / 128.0), reads=[ps_sd[0]], writes=[rind[i]])
                c.op("dve", lambda e: e.tensor_scalar(out=rin[i][:, :n], in0=rin[i][:, :n], scalar1=EPS, scalar2=None, op0=ALU.add), reads=[rind[i]], writes=[rind[i]])
                c.op("act", lambda e: e.activation(out=rin[i][:, :n], in_=rin[i][:, :n], func=AF.Sqrt), reads=[rind[i]], writes=[rind[i]])
                c.op("dve", lambda e: e.reciprocal(out=rin[i][:, :n], in_=rin[i][:, :n]), reads=[rind[i]], writes=[rind[i]])
                if not rope:
                    c.op("dve", lambda e: e.scalar_tensor_tensor(out=dst[:, dsl], in0=ps[:, :n], scalar=gn[:, gcol:gcol + 1], in1=rin[i][:, :n],
                                                                 op0=ALU.mult, op1=ALU.mult), reads=[psd, d_gn, rind[i]], pwrites=[dstd])
                    return
                c.op("dve", lambda e: e.scalar_tensor_tensor(out=qn[i][:, :n], in0=ps[:, :n], scalar=gn[:, gcol:gcol + 1], in1=rin[i][:, :n],
                                                             op0=ALU.mult, op1=ALU.mult), reads=[psd, d_gn, rind[i]], writes=[qnd[i]])
                c.op("pe", lambda e: e.matmul(ps_r[0][:, :n], rm_sb[:], qn[i][:, :n], start=True, stop=True), reads=[d_rm, qnd[i]], writes=[ps_rd[0]])
                c.op("pool", lambda e: e.tensor_tensor(out=t1[i][:, :n], in0=qn[i][:, :n].bitcast(F32), in1=cs_sb[:, dsl], op=ALU.mult), reads=[qnd[i], d_cs], writes=[t1d[i]])
                c.op("dve", lambda e: e.tensor_tensor(out=t2[i][:, :n], in0=ps_r[0][:, :n], in1=sn_sb[:, dsl], op=ALU.mult), reads=[ps_rd[0], d_cs], writes=[t2d[i]])
                c.op("dve", lambda e: e.tensor_tensor(out=dst[:, dsl], in0=t1[i][:, :n], in1=t2[i][:, :n], op=ALU.add), reads=[t1d[i], t2d[i]], pwrites=[dstd])

            state = {}

            def epi(ct, j, ps, psd, n):
                half_sl = slice(j * 512, j * 512 + n)
                if ct < 16:
                    i = ct % 2
                    if j == 0:
                        c.op("dve", lambda e: e.memset(qo[i][:, 0:2], 0.0), writes=[qod[i]])
                    normrope(ps, psd, n, 0, True, qo[i], qod[i], half_sl)
                    if j == 1:
                        P.store("sp", "QT", QT[ct], qo[i][:], qod[i])
                elif ct < 20:
                    g = ct - 16
                    i = ct % 2
                    if j == 0:
                        c.op("dve", lambda e: e.memset(qo[i][:, 0:2], 0.0), writes=[qod[i]])
                    if j < 2:
                        normrope(ps, psd, n, 1, True, qo[i], qod[i], half_sl)
                        if j == 1:
                            P.store("sp", "KTl", KTl[g], qo[i][:], qod[i])
                    else:
                        i2 = (ct + 1) % 2
                        c.op("dve", lambda e: e.memset(qo[i2][:, 0:2], 0.0), writes=[qod[i2]])
                        normrope(ps, psd, n, 1, False, qo[i2], qod[i2], slice(0, 256))
                        P.store("sp", "KTc", KTc[g], qo[i2][:, 0:256], qod[i2])
                else:
                    a = ct - 24
                    jj, isgate = a // 2, a % 2
                    esl = slice([0, 512, 1024][j], [0, 512, 1024][j] + n)
                    if not isgate:
                        c.op("act", lambda e: e.copy(valb[:, esl], ps[:, :n]), reads=[psd], pwrites=[d_valb])
                    else:
                        i = jj % 2
                        c.op("act", lambda e: e.activation(out=sig[:, esl], in_=ps[:, :n], func=AF.Sigmoid), reads=[psd], pwrites=[d_sig])
                        if j == 0:
                            c.op("dve", lambda e: e.memset(gb[i][:, 0:2].bitcast(F32), 0.0), writes=[gbd[i]])
                        c.op("dve", lambda e: e.tensor_tensor(out=sig[:, esl], in0=sig[:, esl], in1=valb[:, esl], op=ALU.mult), reads=[d_sig, d_valb], pwrites=[d_sig])
                        c.op("dve", lambda e: e.tensor_tensor(out=gb[i][:, esl], in0=sig[:, esl], in1=hm[:, esl], op=ALU.mult), reads=[d_sig, d_hm], pwrites=[gbd[i]])
                        if j == 2:
                            P.store("sp", "Gd", Gd[jj], gb[i][:], gbd[i])
                            c.op("act", lambda e: e.copy(valb[:, 0:2], valb[:, 0:2]), reads=[gbd[i]], writes=[d_valb])
                            c.op("act", lambda e: e.copy(sig[:, 0:2], sig[:, 0:2]), reads=[gbd[i]], writes=[d_sig])

            hT_main = hT[:, :, 16:16 + T]
            gemm_fm(P, ph, "gq", "w_in0", w_in0, 16, hT_main, hTd, [(0, 512), (512, 512)], epi)
            def epi_k(ct, j, ps, psd, n):
                epi(ct, j, ps, psd, n)
            if not FUSED:
              gemm_fm(P, ph, "gk", "w_in0", w_in0, 20, hT_main, hTd, [(0, 512), (512, 512)], epi_k, ct_order=[16, 17, 18, 19])
              gemm_fm(P, ph, "gkc", "w_in0", w_in0, 20, hcT, hcTd, [(0, 256)], lambda ct, j, ps, psd, n: epi(ct, 2, ps, psd, n), ct_order=[16, 17, 18, 19])
            vs = ExitStack()
            ws_v = [c.sbuf("wv", [128, NCH, 128], BF16, vs) for _ in range(2)]
            NVG = 0 if FUSED else 4
            wvd = deps(2)
            pv = [c.psum("pv", [128, 512], F32, vs) for _ in range(2)]
            pvd = deps(2)
            nv = 0
            for g in range(NVG):
                s = g % 2
                P.load("pool", ws_v[s][:], "w_in0", w_in0[20 + g], wvd[s], **cast_dma_kw())
                for tt in range(10):
                    b = nv % 2
                    for k in range(NCH):
                        if tt < 8:
                            lhs = hT[:, k, 16 + tt * 128:16 + (tt + 1) * 128]
                            rd = hTd[k]
                        else:
                            lhs = hcT[:, k, (tt - 8) * 128:(tt - 7) * 128]
                            rd = hcTd[k]
                        c.op("pe", lambda e: e.matmul(pv[b][:, 0:128], lhs, ws_v[s][:, k, :], start=(k == 0), stop=(k == NCH - 1)),
                             reads=[rd, wvd[s]], writes=[pvd[b]], inc=(k == NCH - 1))
                    c.op("act", lambda e: e.copy(vo[b][:], pv[b][:, 0:128]), reads=[pvd[b]], writes=[vod[b]])
                    if tt < 8:
                        P.store("sp", "Vl", Vl[g, tt * 128:(tt + 1) * 128, :], vo[b][:], vod[b])
                    else:
                        P.store("sp", "Vc", Vc[g, (tt - 8) * 128:(tt - 7) * 128, :], vo[b][:], vod[b])
                    nv += 1
            c.barrier()
            vs.close()
            gemm_fm(P, ph, "ga", "w_in0", w_in0, 56, hT, hTd, [(0, 512), (512, 512), (1024, 32)], epi, ct_order=list(range(24, 56)))
            c.barrier()
        with ExitStack() as ph:
            cp = c.sbuf("cp", [128, 16, 34], F32, ph); d_cp = Dep()
            P.load("sp", cp[:], "convp", convp, d_cp)
            idn = c.sbuf("idn", [128, 128], F32, ph); d_idn = Dep()
            P.load("sp", idn[:], "ident_r", ident_r.bitcast(F32), d_idn)
            on_sb = c.sbuf("ones", [128, 128], F32R, ph); d_on = Dep()
            P.load("sp", on_sb[:], "ones_r", ones_r, d_on)
            hcv = c.sbuf("hcv", [128, 16, T], F32R, ph); hcvd = deps(16)
            gl = [c.sbuf("gl", [128, EXT], F32R, ph) for _ in range(2)]; gld = deps(2)
            dg = [c.sbuf("dg", [128, 128], F32R, ph) for _ in range(4)]; dgd = deps(4)
            pc = [c.psum("pc", [128, 512], F32, ph) for _ in range(4)]; pcd = deps(4)
            pst = [c.psum("pst", [128, 512], F32, ph) for _ in range(4)]; pstd = deps(4)
            sqb = [c.sbuf("sqb", [128, T], F32R, ph) for _ in range(2)]; sqbd = deps(2)
            nd = 0
            for jj in range(16):
                s = jj % 2
                P.load("sp", gl[s][:], "Gd", Gd[jj], gld[s])
                for k in range(31):
                    di = nd % 4
                    nd += 1
                    c.op("dve", lambda e: e.tensor_scalar(out=dg[di][:], in0=idn[:], scalar1=cp[:, jj, k:k + 1], scalar2=None, op0=ALU.mult),
                         reads=[d_idn, d_cp], writes=[dgd[di]])
                    for half in range(2):
                        pb = (jj % 2) * 2 + half
                        c.op("pe", lambda e: e.matmul(pc[pb][:], dg[di][:], gl[s][:, 1 + k + half * 512:1 + k + half * 512 + 512],
                                                      start=(k == 0), stop=(k == 30)),
                             reads=[dgd[di], gld[s]], writes=[pcd[pb]], inc=True)
                for half in range(2):
                    pb = (jj % 2) * 2 + half
                    sl = slice(half * 512, half * 512 + 512)
                    c.op("act", lambda e: e.activation(out=hcv[:, jj, sl], in_=pc[pb][:], func=AF.Identity, bias=cp[:, jj, 31:32], scale=1.0),
                         reads=[pcd[pb], d_cp], pwrites=[hcvd[jj]])
                q = jj % 2
                c.op("act", lambda e: e.activation(out=sqb[q][:], in_=hcv[:, jj, :], func=AF.Square), reads=[hcvd[jj]], writes=[sqbd[q]])
                for half in range(2):
                    sl = slice(half * 512, half * 512 + 512)
                    c.op("pe", lambda e: e.matmul(pst[half][:], on_sb[:], hcv[:, jj, sl], start=(jj == 0), stop=(jj == 15)),
                         reads=[d_on, hcvd[jj]], writes=[pstd[half]], inc=True)
                    c.op("pe", lambda e: e.matmul(pst[2 + half][:], on_sb[:], sqb[q][:, sl], start=(jj == 0), stop=(jj == 15)),
                         reads=[d_on, sqbd[q]], writes=[pstd[2 + half]], inc=True)
            mean = c.sbuf("cmean", [128, T], F32, ph)
            rstd = c.sbuf("crstd", [128, T], F32, ph)
            nmr = c.sbuf("cnmr", [128, T], F32, ph)
            tmp = c.sbuf("ctmp", [128, T], F32, ph)
            d_mean, d_rstd, d_nmr, d_tmp = deps(4)
            CW = 2048.0
            for half in range(2):
                sl = slice(half * 512, half * 512 + 512)
                c.op("act", lambda e: e.mul(mean[:, sl], pst[half][:], 1.0 / CW), reads=[pstd[half]], pwrites=[d_mean])
                c.op("dve", lambda e: e.tensor_tensor(out=tmp[:, sl], in0=mean[:, sl], in1=mean[:, sl], op=ALU.mult), reads=[d_mean], pwrites=[d_tmp])
                c.op("dve", lambda e: e.scalar_tensor_tensor(out=rstd[:, sl], in0=pst[2 + half][:], scalar=1.0 / CW, in1=tmp[:, sl],
                                                             op0=ALU.mult, op1=ALU.subtract), reads=[pstd[2 + half], d_tmp], pwrites=[d_rstd])
                c.op("dve", lambda e: e.tensor_scalar(out=rstd[:, sl], in0=rstd[:, sl], scalar1=EPS, scalar2=None, op0=ALU.add), reads=[d_rstd], pwrites=[d_rstd])
                c.op("act", lambda e: e.activation(out=rstd[:, sl], in_=rstd[:, sl], func=AF.Sqrt), reads=[d_rstd], pwrites=[d_rstd])
                c.op("dve", lambda e: e.reciprocal(out=rstd[:, sl], in_=rstd[:, sl]), reads=[d_rstd], pwrites=[d_rstd])
                c.op("dve", lambda e: e.scalar_tensor_tensor(out=nmr[:, sl], in0=mean[:, sl], scalar=-1.0, in1=rstd[:, sl],
                                                             op0=ALU.mult, op1=ALU.mult), reads=[d_mean, d_rstd], pwrites=[d_nmr])
            xn = [c.sbuf("cxn", [128, T], F32, ph) for _ in range(2)]; xnd = deps(2)
            fo = [c.sbuf("cfo", [128, T], BF16, ph) for _ in range(2)]; fod = deps(2)
            for jj in range(16):
                q = jj % 2
                c.op("dve", lambda e: e.tensor_tensor(out=xn[q][:], in0=hcv[:, jj, :].bitcast(F32), in1=rstd[:], op=ALU.mult), reads=[hcvd[jj], d_rstd], writes=[xnd[q]])
                c.op("pool", lambda e: e.tensor_tensor(out=xn[q][:], in0=xn[q][:], in1=nmr[:], op=ALU.add), reads=[xnd[q], d_nmr], writes=[xnd[q]])
                c.op("dve", lambda e: e.tensor_scalar(out=xn[q][:], in0=xn[q][:], scalar1=cp[:, jj, 32:33], scalar2=cp[:, jj, 33:34],
                                                      op0=ALU.mult, op1=ALU.add), reads=[xnd[q], d_cp], writes=[xnd[q]])
                c.op("act", lambda e: e.activation(out=fo[q][:], in_=xn[q][:], func=AF.Silu), reads=[xnd[q]], writes=[fod[q]])
                P.store("sp", "featc", featc[jj], fo[q][:], fod[q])
            c.barrier()
        if not FUSED:
            c.finish()
            return nc

    assert stage in ("rest", "all")
    if not FUSED:
        xT = P.din("xT", [128, NCH, EXT], F32)
        QT = P.din("QT", [16, 128, T], BF16)
        KTa = P.din("KTa", [4, 128, 8448], BF16)
        Va = P.din("Va", [4, 128, 66, 128], BF16)
        featc = P.din("featc", [16, 128, T], BF16)
    lnp = P.din("lnp", [4, 128, 2, NCH], F32)
    P.dram.update({"lnp%d" % i: lnp[i] for i in range(4)})
    for i in range(4):
        P.ddeps["lnp%d" % i] = P.ddeps["lnp"]
    w_out = [P.din("w_out%d" % l, [32, 128, NCH, 128], F32) for l in range(2)]
    wq = [P.din("wq%d" % l, [16, 128, NCH, 128], F32) for l in range(2)]
    k1T = [P.din("k1T%d" % l, [128, 8, 128], F32R) for l in range(2)]
    k2T = [P.din("k2T%d" % l, [128, 8, 128], F32R) for l in range(2)]
    UT = [P.din("UT%d" % l, [128, 128, NCH, 128], F32) for l in range(2)]
    VR = [P.din("VR%d" % l, [8, 128, 128, 512], F32) for l in range(2)]
    w1u = P.din("w1u", [32, 128, NCH, 128], F32)
    w1v = P.din("w1v", [8, 128, NCH, 512], F32)
    sgu_gb = P.din("sgu_gb", [2, D], F32)
    sguWT = P.din("sguWT", [128, 16, 128], F32)
    sgub2 = P.din("sgub2", [1, 32 * 128], F32)
    outT = P.dout("outT", [NCH, 128, T], F32)
    featA = P.dint("featA", [16, 128, T], BF16)
    uS = P.dint("uS", [NCH, 128, T], F32R)
    x1T = P.dint("x1T", [NCH, 128, T], F32)
    x2T = P.dint("x2T", [NCH, 128, T], F32)
    x3T = P.dint("x3T", [NCH, 128, T], F32)
    actT = P.dint("actT", [128, 128, T], BF16)
    WTd = P.dint("WTd", [128, 128, T], BF16)
    uT1 = P.dint("uT1", [NCH, 128, T], BF16)
    vtok = P.dint("vtok", [8, 128, D], F32)
    featB = P.dint("featB", [NCH, 128, T], BF16)

    class Resid:
        def __init__(self, ph, pfx, l, gmod, xname, xfn):
            self.l, self.gmod, self.xname, self.xfn = l, gmod, xname, xfn
            self.xs = [c.sbuf(pfx + "rx", [128, T], F32, ph) for _ in range(2)]
            self.xsd = deps(2)
            self.yg = [c.sbuf(pfx + "ryg", [128, 512], F32, ph) for _ in range(2)]
            self.ygd = deps(2)
            self.ut = [c.sbuf(pfx + "rut", [128, T], F32R, ph) for _ in range(2)]
            self.utd = deps(2)
            self.n = 0
            self.m = 0

        def __call__(self, ct, half, ps, psd):
            i = self.n % 2
            sl = slice(half * 512, half * 512 + 512)
            if half == 0:
                P.load("sp", self.xs[i][:], self.xname, self.xfn(ct), self.xsd[i])
            j = self.m % 2
            self.m += 1
            gcol = self.gmod * NCH + ct
            c.op("act", lambda e: e.activation(out=self.yg[j][:], in_=ps[:, :512], func=AF.Identity, scale=mod_sb[:, self.l, 0, gcol:gcol + 1]),
                 reads=[psd, P.d_mod], writes=[self.ygd[j]])
            c.op("dve", lambda e: e.scalar_tensor_tensor(out=self.ut[i][:, sl], in0=self.xs[i][:, sl], scalar=ALPHA, in1=self.yg[j][:],
                                                         op0=ALU.mult, op1=ALU.add), reads=[self.xsd[i], self.ygd[j]], pwrites=[self.utd[i]])
            if half == 1:
                P.store("sp", "uS", uS[ct], self.ut[i][:], self.utd[i])
                self.n += 1

    with ExitStack() as ph:
        kt = [c.sbuf("kt", [128, 8448], BF16, ph) for _ in range(2)]; ktd = deps(2)
        vt = [c.sbuf("vt", [128, 66, 128], BF16, ph) for _ in range(2)]; vtd = deps(2)
        qt = [c.sbuf("qt", [128, T], BF16, ph) for _ in range(2)]; qtd = deps(2)
        pt = [c.sbuf("pt", [128, 512], BF16, ph) for _ in range(4)]; ptd = deps(4)
        onb = c.sbuf("onb", [128, 128], BF16, ph); d_onb = Dep()
        c.op("dve", lambda e: e.memset(onb[:], 1.0), writes=[d_onb])
        ob = [c.sbuf("ob", [128, T], BF16, ph) for _ in range(2)]; obd = deps(2)
        rd = [c.sbuf("rd", [128, 512], F32, ph) for _ in range(2)]; rdd = deps(2)
        pS = [c.psum("pS", [128, 512], F32, ph) for _ in range(3)]; pSd = deps(3)
        pO = [c.psum("pO", [128, 512], F32, ph) for _ in range(2)]; pOd = deps(2)
        pD = [c.psum("pD", [128, 512], F32, ph) for _ in range(2)]; pDd = deps(2)
        SCALE = 128.0 ** -0.5
        ns = 0
        for h in range(16):
            g = h // 4
            gs = g % 2
            if h % 4 == 0:
                P.load("sp", kt[gs][:], "KTa", KTa[g], ktd[gs])
                P.load("sp", vt[gs][:], "Va", Va[g], vtd[gs])
            qi = h % 2
            P.load("sp", qt[qi][:], "QT", QT[h], qtd[qi])
            for half in range(2):
                ab = (h * 2 + half) % 2
                hsl = slice(half * 512, half * 512 + 512)
                def s_and_exp(st):
                    sb = (ns + st) % 3
                    pi = (ns + st) % 4
                    c.op("pe", lambda e: e.matmul(pS[sb][:], kt[gs][:, st * 128:(st + 1) * 128], qt[qi][:, hsl], start=True, stop=True),
                         reads=[ktd[gs], qtd[qi]], writes=[pSd[sb]])
                    c.op("act", lambda e: e.activation(out=pt[pi][:], in_=pS[sb][:], func=AF.Exp, scale=SCALE), reads=[pSd[sb]], writes=[ptd[pi]])

                def pv_den(st):
                    pi = (ns + st) % 4
                    c.op("pe", lambda e: e.matmul(pO[ab][:], vt[gs][:, st, :], pt[pi][:], start=(st == 0), stop=(st == 65)),
                         reads=[vtd[gs], ptd[pi]], writes=[pOd[ab]], inc=False)
                    c.op("pe", lambda e: e.matmul(pD[ab][:], onb[:], pt[pi][:], start=(st == 0), stop=(st == 65)),
                         reads=[d_onb, ptd[pi]], writes=[pDd[ab]], inc=True)
                LOOK = 2
                for st in range(min(LOOK, 66)):
                    s_and_exp(st)
                for st in range(66):
                    if st + LOOK < 66:
                        s_and_exp(st + LOOK)
                    pv_den(st)
                ns += 66
                ri = (h * 2 + half) % 2
                c.op("dve", lambda e: e.reciprocal(out=rd[ri][:], in_=pD[ab][:]), reads=[pDd[ab]], writes=[rdd[ri]])
                c.op("dve", lambda e: e.tensor_tensor(out=ob[qi][:, hsl], in0=pO[ab][:], in1=rd[ri][:], op=ALU.mult), reads=[pOd[ab], rdd[ri]], pwrites=[obd[qi]])
            P.store("sp", "featA", featA[h], ob[qi][:], obd[qi])
        c.barrier()

    def load_feat(ft, d0, d1, nameA, apA, nameB, apB):
        for ch in range(16):
            P.load("sp", ft[:, ch, :], nameA, apA[ch], d0, partial=True)
        for ch in range(16):
            P.load("sp", ft[:, 16 + ch, :], nameB, apB[ch], d1, partial=True)

    def wout_phase(pfx, l, feat_loader, xname, xfn):
        with ExitStack() as ph:
            ft = c.sbuf(pfx + "ft", [128, NCH, T], BF16, ph)
            d0, d1 = deps(2)
            feat_loader(ft, d0, d1)
            rs = Resid(ph, pfx, l, 2, xname, xfn)
            gemm_fm(P, ph, pfx + "g", "w_out%d" % l, w_out[l], 32, ft, [d0] * 16 + [d1] * 16, [(0, 512), (512, 512)],
                    lambda ct, j, ps, psd, n: rs(ct, j, ps, psd))
            c.barrier()

    def peer(l, hT, hTd, hstack, qstack, qT, xname, xd):
        pfx = "pr%d" % l
        with ExitStack() as ph:
            ao = [c.sbuf(pfx + "ao", [128, T], BF16, ph) for _ in range(2)]; aod = deps(2)

            def epi(ct, j, ps, psd, n):
                i = ct % 2
                c.op("act", lambda e: e.activation(out=ao[i][:, j * 512:(j + 1) * 512], in_=ps[:, :512], func=GELU), reads=[psd], pwrites=[aod[i]])
                if j == 1:
                    P.store("sp", "actT", actT[ct], ao[i][:], aod[i])
            gemm_fm(P, ph, pfx + "a1", "UT%d" % l, UT[l], 128, hT, hTd, [(0, 512), (512, 512)], epi)
            c.barrier()
        qTd = deps(16)
        with ExitStack() as ph:
            def epi(ct, j, ps, psd, n):
                c.op("act", lambda e: e.copy(qT[:, ct, j * 512:(j + 1) * 512], ps[:, :512]), reads=[psd], pwrites=[qTd[ct]])
            gemm_fm(P, ph, pfx + "wq", "wq%d" % l, wq[l], 16, hT, hTd, [(0, 512), (512, 512)], epi)
            c.barrier()
        hstack.close()
        S2a = c.sbuf(pfx + "S2a", [128, 8, T], F32R, qstack); d_S2a = deps(8)
        S2b = c.sbuf(pfx + "S2b", [128, 8, T], F32R, qstack); d_S2b = deps(8)
        k1s = c.sbuf(pfx + "k1", [128, 8, 128], F32R, qstack); d_k1 = Dep()
        k2s = c.sbuf(pfx + "k2", [128, 8, 128], F32R, qstack); d_k2 = Dep()
        idr = c.sbuf(pfx + "idr", [128, 128], F32R, qstack); d_idr = Dep()
        P.load("sp", k1s[:], "k1T%d" % l, k1T[l], d_k1)
        P.load("sp", k2s[:], "k2T%d" % l, k2T[l], d_k2)
        P.load("sp", idr[:], "ident_r", ident_r, d_idr)
        with ExitStack() as ph:
            S2 = c.sbuf(pfx + "S2", [128, 8, T], F32, ph); d_S2 = deps(8)
            idf = c.sbuf(pfx + "idf", [128, 128], F32, ph); d_idf = Dep()
            P.load("sp", idf[:], "ident_r", ident_r.bitcast(F32), d_idf)
            ST = c.sbuf(pfx + "ST", [128, 8, 16], F32, ph); d_ST = deps(8)
            STT = c.sbuf(pfx + "STT", [16, T], F32R, ph); d_STT = Dep()
            pA = [c.psum(pfx + "pA", [128, 512], F32, ph) for _ in range(2)]; pAd = deps(2)
            pB = [c.psum(pfx + "pB", [128, 512], F32, ph) for _ in range(2)]; pBd = deps(2)
            n = 0
            for h in range(8):
                for half in range(2):
                    b = n % 2; n += 1
                    sl = slice(half * 512, half * 512 + 512)
                    c.op("pe", lambda e: e.matmul(pA[b][:], k2s[:, h, :], qT[:, 2 * h + 1, sl], start=True, stop=True), reads=[d_k2, qTd[2 * h + 1]], writes=[pAd[b]])
                    c.op("act", lambda e: e.copy(S2[:, h, sl], pA[b][:]), reads=[pAd[b]], pwrites=[d_S2[h]])
            sc = [c.sbuf(pfx + "sc", [128, 256], F32, ph) for _ in range(2)]; scd = deps(2)
            tm = [c.sbuf(pfx + "tm", [128, 256], F32, ph) for _ in range(2)]; tmd = deps(2)
            tm2 = [c.sbuf(pfx + "tm2", [128, 256], F32, ph) for _ in range(2)]; tm2d = deps(2)
            vv = [c.sbuf(pfx + "vv", [128, 32], F32, ph) for _ in range(2)]; vvd = deps(2)
            cd = [c.sbuf(pfx + "cd", [128, 16, 16], F32, ph) for _ in range(2)]; cdd = deps(2)
            mm = [c.sbuf(pfx + "mm", [128, 24], F32, ph) for _ in range(2)]; mmd = deps(2)
            sm = [c.sbuf(pfx + "sm", [128, 8], F32, ph) for _ in range(2)]; smd = deps(2)
            ex = [c.sbuf(pfx + "ex", [128, 16], F32, ph) for _ in range(2)]; exd = deps(2)
            n = 0
            for tt in range(8):
                tsl = slice(tt * 128, tt * 128 + 128)
                for h in range(8):
                    b = n % 2; n += 1
                    c.op("pe", lambda e: e.matmul(pB[b][:, 0:128], qT[:, 2 * h, tsl], k1s[:, h, :], start=True, stop=True), reads=[qTd[2 * h], d_k1], writes=[pBd[b]])
                    c.op("pe", lambda e: e.matmul(pB[b][:, 128:256], qT[:, 2 * h + 1, tsl], k2s[:, h, :], start=True, stop=True), reads=[qTd[2 * h + 1], d_k2], writes=[pBd[b]])
                    c.op("act", lambda e: e.copy(sc[b][:], pB[b][:, 0:256]), reads=[pBd[b]], writes=[scd[b]])
                    for w_ in range(2):
                        o = w_ * 128
                        c.op("dve", lambda e: e.max(out=vv[b][:, w_ * 16:w_ * 16 + 8], in_=sc[b][:, o:o + 128]), reads=[scd[b]], pwrites=[vvd[b]])
                        c.op("dve", lambda e: e.match_replace(out=tm[b][:, o:o + 128], in_to_replace=vv[b][:, w_ * 16:w_ * 16 + 8], in_values=sc[b][:, o:o + 128], imm_value=NEG),
                             reads=[scd[b], vvd[b]], pwrites=[tmd[b]])
                        c.op("dve", lambda e: e.max(out=vv[b][:, w_ * 16 + 8:w_ * 16 + 16], in_=tm[b][:, o:o + 128]), reads=[tmd[b]], pwrites=[vvd[b]])
                    c.op("dve", lambda e: e.tensor_tensor(out=cd[b][:], in0=vv[b][:, 0:16].unsqueeze(2).to_broadcast([128, 16, 16]),
                                                          in1=vv[b][:, 16:32].unsqueeze(1).to_broadcast([128, 16, 16]), op=ALU.add), reads=[vvd[b]], writes=[cdd[b]])
                    cflat = cd[b][:].rearrange("p a b -> p (a b)")
                    c.op("dve", lambda e: e.max(out=mm[b][:, 0:8], in_=cflat), reads=[cdd[b]], pwrites=[mmd[b]])
                    c.op("dve", lambda e: e.match_replace(out=tm[b][:], in_to_replace=mm[b][:, 0:8], in_values=cflat, imm_value=NEG), reads=[cdd[b], mmd[b]], writes=[tmd[b]])
                    c.op("dve", lambda e: e.max(out=mm[b][:, 8:16], in_=tm[b][:]), reads=[tmd[b]], pwrites=[mmd[b]])
                    c.op("dve", lambda e: e.match_replace(out=tm2[b][:], in_to_replace=mm[b][:, 8:16], in_values=tm[b][:], imm_value=NEG), reads=[tmd[b], mmd[b]], writes=[tm2d[b]])
                    c.op("dve", lambda e: e.max(out=mm[b][:, 16:24], in_=tm2[b][:]), reads=[tm2d[b]], pwrites=[mmd[b]])
                    c.op("dve", lambda e: e.tensor_tensor(out=sm[b][:, 0:1], in0=mm[b][:, 15:16], in1=mm[b][:, 16:17], op=ALU.add), reads=[mmd[b]], pwrites=[smd[b]])
                    c.op("dve", lambda e: e.tensor_scalar(out=ST[:, tt, h:h + 1], in0=sm[b][:, 0:1], scalar1=-0.5, scalar2=None, op0=ALU.mult), reads=[smd[b]], pwrites=[d_ST[tt]])
                    c.op("dve", lambda e: e.tensor_scalar(out=sm[b][:, 1:2], in0=mm[b][:, 0:1], scalar1=-1.0, scalar2=None, op0=ALU.mult), reads=[mmd[b]], pwrites=[smd[b]])
                    c.op("act", lambda e: e.activation(out=ex[b][:], in_=mm[b][:, 0:16], func=AF.Exp, bias=sm[b][:, 1:2], scale=1.0, accum_out=sm[b][:, 2:3]),
                         reads=[mmd[b], smd[b]], writes=[exd[b]], pwrites=[smd[b]])
                    c.op("act", lambda e: e.activation(out=sm[b][:, 3:4], in_=sm[b][:, 2:3], func=AF.Ln), reads=[smd[b], exd[b]], pwrites=[smd[b]])
                    c.op("dve", lambda e: e.tensor_tensor(out=ST[:, tt, 8 + h:9 + h], in0=sm[b][:, 1:2], in1=sm[b][:, 3:4], op=ALU.subtract), reads=[smd[b]], pwrites=[d_ST[tt]])
            for tt in range(8):
                b = tt % 2
                c.op("pe", lambda e: e.transpose(out=pA[b][0:16, 0:128], in_=ST[:, tt, :], identity=idf[:]), reads=[d_ST[tt], d_idf], writes=[pAd[b]])
                c.op("dve", lambda e: e.tensor_copy(out=STT[:, tt * 128:(tt + 1) * 128], in_=pA[b][0:16, 0:128]), reads=[pAd[b]], pwrites=[d_STT])
            n = 0
            for h in range(8):
                for half in range(2):
                    sl = slice(half * 512, half * 512 + 512)
                    for which, dst, dd in ((0, S2a, d_S2a), (1, S2b, d_S2b)):
                        b = n % 2; n += 1
                        r_ = which * 8 + h
                        c.op("pe", lambda e: e.matmul(pA[b][:], idr[0:16, r_:r_ + 1].to_broadcast([16, 128]), STT[:, sl], start=True, stop=True),
                             reads=[d_idr, d_STT], writes=[pAd[b]])
                        c.op("dve", lambda e: e.tensor_tensor(out=dst[:, h, sl], in0=pA[b][:], in1=S2[:, h, sl], op=ALU.add), reads=[pAd[b], d_S2[h]], pwrites=[dd[h]])
            c.barrier()
        with ExitStack() as ph:
            at = [c.sbuf(pfx + "at", [128, T], BF16, ph) for _ in range(2)]; atd = deps(2)
            pe_ = [c.sbuf(pfx + "pe", [128, 512], F32, ph) for _ in range(3)]; ped = deps(3)
            tp = [c.sbuf(pfx + "tp", [128, 512], F32, ph) for _ in range(3)]; tpd = deps(3)
            acc = [c.sbuf(pfx + "acc", [128, T], F32, ph) for _ in range(2)]; accd = [deps(2), deps(2)]
            wo = [c.sbuf(pfx + "wo", [128, T], BF16, ph) for _ in range(2)]; wod = deps(2)
            pA = [c.psum(pfx + "qA", [128, 512], F32, ph) for _ in range(4)]; pAd = deps(4)
            pB = [c.psum(pfx + "qB", [128, 512], F32, ph) for _ in range(4)]; pBd = deps(4)
            n = 0
            for i in range(128):
                ai = i % 2
                P.load("sp", at[ai][:], "actT", actT[i], atd[ai])
                for h in range(8):
                    for half in range(2):
                        b = n % 4
                        t3 = n % 3
                        n += 1
                        sl = slice(half * 512, half * 512 + 512)
                        lhs = k1s[:, h, i:i + 1].to_broadcast([128, 128])
                        c.op("pe", lambda e: e.matmul(pA[b][:], lhs, qT[:, 2 * h, sl], start=True, stop=False), reads=[d_k1, qTd[2 * h]], writes=[pAd[b]], inc=False)
                        c.op("pe", lambda e: e.matmul(pA[b][:], idr[:], S2a[:, h, sl], start=False, stop=True), reads=[d_idr, d_S2a[h]], writes=[pAd[b]], inc=True)
                        c.op("pe", lambda e: e.matmul(pB[b][:], lhs, qT[:, 2 * h, sl], start=True, stop=False), reads=[d_k1, qTd[2 * h]], writes=[pBd[b]], inc=False)
                        c.op("pe", lambda e: e.matmul(pB[b][:], idr[:], S2b[:, h, sl], start=False, stop=True), reads=[d_idr, d_S2b[h]], writes=[pBd[b]], inc=True)
                        c.op("act", lambda e: e.activation(out=pe_[t3][:], in_=pB[b][:], func=AF.Exp), reads=[pBd[b]], writes=[ped[t3]])
                        if h == 0:
                            c.op("dve", lambda e: e.scalar_tensor_tensor(out=acc[ai][:, sl], in0=pA[b][:], scalar=0.0, in1=pe_[t3][:], op0=ALU.is_ge, op1=ALU.mult),
                                 reads=[pAd[b], ped[t3]], writes=[accd[ai][half]])
                        else:
                            c.op("dve", lambda e: e.scalar_tensor_tensor(out=tp[t3][:], in0=pA[b][:], scalar=0.0, in1=pe_[t3][:], op0=ALU.is_ge, op1=ALU.mult),
                                 reads=[pAd[b], ped[t3]], writes=[tpd[t3]])
                            c.op("pool", lambda e: e.tensor_tensor(out=acc[ai][:, sl], in0=acc[ai][:, sl], in1=tp[t3][:], op=ALU.add),
                                 reads=[tpd[t3], accd[ai][half]], writes=[accd[ai][half]])
                c.op("dve", lambda e: e.tensor_tensor(out=wo[ai][:], in0=acc[ai][:], in1=at[ai][:], op=ALU.mult), reads=[accd[ai][0], accd[ai][1], atd[ai]], writes=[wod[ai]])
                P.store("sp", "WTd", WTd[i], wo[ai][:], wod[ai])
            c.barrier()
        qstack.close()
        with ExitStack() as ph:
            rs = Resid(ph, pfx + "b", l, 5, xname, lambda ct: xd[ct])
            wt = [c.sbuf(pfx + "wt", [128, T], BF16, ph) for _ in range(8)]; wtd = deps(8)
            vs_ = [c.sbuf(pfx + "vs", [128, 512], BF16, ph) for _ in range(8)]; vsd = deps(8)
            pp = [c.psum(pfx + "pp", [128, 512], F32, ph) for _ in range(8)]; ppd = deps(8)
            n = 0
            for p_ in range(8):
                for et in range(128):
                    s = n % 8; n += 1
                    P.load("sp", wt[s][:], "WTd", WTd[et], wtd[s])
                    P.load("pool", vs_[s][:], "VR%d" % l, VR[l][p_, et], vsd[s], **cast_dma_kw())
                    for dc in range(4):
                        for half in range(2):
                            b = dc * 2 + half
                            c.op("pe", lambda e: e.matmul(pp[b][:], vs_[s][:, dc * 128:(dc + 1) * 128], wt[s][:, half * 512:(half + 1) * 512],
                                                          start=(et == 0), stop=(et == 127)),
                                 reads=[vsd[s], wtd[s]], writes=[ppd[b]], inc=(et == 127 or (dc == 3 and half == 1)))
                for dc in range(4):
                    for half in range(2):
                        rs(p_ * 4 + dc, half, pp[dc * 2 + half], ppd[dc * 2 + half])
            c.barrier()

    wout_phase("wo0", 0, lambda ft, d0, d1: load_feat(ft, d0, d1, "featA", featA, "featc", featc), "xT", lambda ct: xT[:, ct, 16:16 + T])
    qs = ExitStack()
    qT = c.sbuf("qT_a", [128, 16, T], F32R, qs)
    hs = ExitStack()
    hT = c.sbuf("hT_a", [128, NCH, T], BF16, hs); hTd = deps(NCH)
    ln_phase(P, "ln0", "uS", "lnp0", "x1T", hT, hTd, mod_sb, 0, 3, 4)
    peer(0, hT, hTd, hs, qs, qT, "x1T", x1T)
    hs = ExitStack()
    hT = c.sbuf("hT_b", [128, NCH, T], BF16, hs); hTd = deps(NCH)
    ln_phase(P, "ln1", "uS", "lnp1", "x2T", hT, hTd, mod_sb, 1, 0, 1)
    with ExitStack() as ph:
        uo = [c.sbuf("g1uo", [128, T], BF16, ph) for _ in range(2)]; uod = deps(2)

        def epi(ct, j, ps, psd, n):
            i = ct % 2
            c.op("act", lambda e: e.activation(out=uo[i][:, j * 512:(j + 1) * 512], in_=ps[:, :512], func=GELU), reads=[psd], pwrites=[uod[i]])
            if j == 1:
                P.store("sp", "uT1", uT1[ct], uo[i][:], uod[i])
        gemm_fm(P, ph, "g1u", "w1u", w1u, 32, hT, hTd, [(0, 512), (512, 512)], epi)
        c.barrier()
    with ExitStack() as ph:
        wv_ = [c.sbuf("g1wv", [128, NCH, 512], BF16, ph) for _ in range(2)]; wvd = deps(2)
        vo_ = [c.sbuf("g1vo", [128, 512], F32, ph) for _ in range(3)]; vod_ = deps(3)
        pv = [c.psum("g1pv", [128, 512], F32, ph) for _ in range(4)]; pvd = deps(4)
        n = 0
        for ct in range(8):
            s = ct % 2
            P.load("pool", wv_[s][:], "w1v", w1v[ct], wvd[s], **cast_dma_kw())
            for tt in range(8):
                b = n % 4; o3 = n % 3; n += 1
                for k in range(NCH):
                    c.op("pe", lambda e: e.matmul(pv[b][:], hT[:, k, tt * 128:(tt + 1) * 128], wv_[s][:, k, :], start=(k == 0), stop=(k == NCH - 1)),
                         reads=[hTd[k], wvd[s]], writes=[pvd[b]], inc=(k == NCH - 1))
                c.op("act", lambda e: e.activation(out=vo_[o3][:], in_=pv[b][:], func=GELU), reads=[pvd[b]], writes=[vod_[o3]])
                P.store("sp", "vtok", vtok[tt][:, ct * 512:(ct + 1) * 512], vo_[o3][:], vod_[o3])
        c.barrier()
    hs.close()
    with ExitStack() as ph:
        uts = c.sbuf("g2u", [128, NCH, T], BF16, ph); d_uts = Dep()
        for ch in range(NCH):
            P.load("sp", uts[:, ch, :], "uT1", uT1[ch], d_uts, partial=True)
        gbc = c.sbuf("g2g", [128, 2, D], F32, ph); d_gbc = Dep()
        P.load("sp", gbc[:, 0, :], "sgu_gb", sgu_gb[0:1, :].partition_broadcast(128), d_gbc, partial=True)
        P.load("sp", gbc[:, 1, :], "sgu_gb", sgu_gb[1:2, :].partition_broadcast(128), d_gbc, partial=True)
        swt = c.sbuf("g2w", [128, 16, 128], BF16, ph); d_swt = Dep()
        P.load("pool", swt[:], "sguWT", sguWT, d_swt, **cast_dma_kw())
        sb2 = c.sbuf("g2b", [128, 32 * 128], F32, ph); d_sb2 = Dep()
        P.load("sp", sb2[:], "sgub2", sgub2.partition_broadcast(128), d_sb2)
        vt_ = [c.sbuf("g2v", [128, D], F32, ph) for _ in range(2)]; vtd_ = deps(2)
        vln = [c.sbuf("g2vl", [128, D], BF16, ph) for _ in range(2)]; vlnd = deps(2)
        bst = [c.sbuf("g2bs", [128, 8, 6], F32, ph) for _ in range(2)]; bstd = deps(2)
        mv = [c.sbuf("g2mv", [128, 4], F32, ph) for _ in range(2)]; mvd = deps(2)
        tmpb = [c.sbuf("g2t", [128, 512], F32, ph) for _ in range(2)]; tmpd = deps(2)
        og = [c.sbuf("g2o", [128, 4, 128], BF16, ph) for _ in range(3)]; ogd = deps(3)
        pm = [c.psum("g2p", [128, 512], F32, ph) for _ in range(4)]; pmd = deps(4)
        n = 0
        for tt in range(8):
            s = tt % 2
            P.load("sp", vt_[s][:], "vtok", vtok[tt], vtd_[s])
            for j in range(8):
                c.op("dve", lambda e: e.bn_stats(out=bst[s][:, j, :], in_=vt_[s][:, j * 512:(j + 1) * 512]), reads=[vtd_[s]], pwrites=[bstd[s]])
            c.op("dve", lambda e: e.bn_aggr(out=mv[s][:, 0:2], in_=bst[s][:].rearrange("p a b -> p (a b)")), reads=[bstd[s]], pwrites=[mvd[s]])
            c.op("dve", lambda e: e.tensor_scalar(out=mv[s][:, 2:3], in0=mv[s][:, 1:2], scalar1=EPS, scalar2=None, op0=ALU.add), reads=[mvd[s]], pwrites=[mvd[s]])
            c.op("act", lambda e: e.activation(out=mv[s][:, 2:3], in_=mv[s][:, 2:3], func=AF.Sqrt), reads=[mvd[s]], pwrites=[mvd[s]])
            c.op("dve", lambda e: e.reciprocal(out=mv[s][:, 3:4], in_=mv[s][:, 2:3]), reads=[mvd[s]], pwrites=[mvd[s]])
            c.op("dve", lambda e: e.tensor_scalar(out=vt_[s][:], in0=vt_[s][:], scalar1=mv[s][:, 0:1], scalar2=mv[s][:, 3:4], op0=ALU.subtract, op1=ALU.mult),
                 reads=[mvd[s]], writes=[vtd_[s]])
            c.op("pool", lambda e: e.tensor_tensor(out=vt_[s][:], in0=vt_[s][:], in1=gbc[:, 0, :], op=ALU.mult), reads=[d_gbc], writes=[vtd_[s]])
            c.op("dve", lambda e: e.tensor_tensor(out=vln[s][:], in0=vt_[s][:], in1=gbc[:, 1, :], op=ALU.add), reads=[vtd_[s], d_gbc], writes=[vlnd[s]])
            for cg in range(8):
                b = n % 4; t2_ = n % 2; o3 = n % 3; n += 1
                for cc in range(4):
                    ch = cg * 4 + cc
                    c.op("pe", lambda e: e.matmul(pm[b][:, cc * 128:(cc + 1) * 128], vln[s][:, ch * 128:(ch + 1) * 128], swt[:, ch // 2, :], start=True, stop=True),
                         reads=[vlnd[s], d_swt], writes=[pmd[b]], inc=(cc == 3))
                c.op("dve", lambda e: e.tensor_tensor(out=tmpb[t2_][:], in0=pm[b][:], in1=sb2[:, cg * 512:(cg + 1) * 512], op=ALU.add), reads=[pmd[b], d_sb2], writes=[tmpd[t2_]])
                c.op("dve", lambda e: e.tensor_tensor(out=og[o3][:], in0=tmpb[t2_][:].rearrange("p (a b) -> p a b", a=4), in1=uts[:, cg * 4:(cg + 1) * 4, tt * 128:(tt + 1) * 128], op=ALU.mult),
                     reads=[tmpd[t2_], d_uts], writes=[ogd[o3]])
                P.store("sp", "featB", featB[cg * 4:(cg + 1) * 4, :, tt * 128:(tt + 1) * 128].rearrange("c p t -> p c t"), og[o3][:], ogd[o3])
        c.barrier()

    def load_featB(ft, d0, d1):
        for ch in range(16):
            P.load("sp", ft[:, ch, :], "featB", featB[ch], d0, partial=True)
        for ch in range(16, 32):
            P.load("sp", ft[:, ch, :], "featB", featB[ch], d1, partial=True)
    wout_phase("wo1", 1, load_featB, "x2T", lambda ct: x2T[ct])
    qs = ExitStack()
    qT = c.sbuf("qT_b", [128, 16, T], F32R, qs)
    hs = ExitStack()
    hT = c.sbuf("hT_c", [128, NCH, T], BF16, hs); hTd = deps(NCH)
    ln_phase(P, "ln2", "uS", "lnp2", "x3T", hT, hTd, mod_sb, 1, 3, 4)
    peer(1, hT, hTd, hs, qs, qT, "x3T", x3T)
    ln_phase(P, "ln3", "uS", "lnp3", "outT")
    c.finish()
    return nc


def fm_vec(v):
    v = np.asarray(v, np.float32).reshape(-1, 128)
    return np.ascontiguousarray(v.T)


def wtiles(w):
    K, N = w.shape
    return np.ascontiguousarray(w.reshape(K // 128, 128, N // 128, 128).transpose(2, 1, 0, 3))


def run(stage, in_maps):
    nc = build(stage)
    res = run_bass_kernel_spmd(nc, in_maps, core_ids=list(range(NCORES)))
    return res.results


def rope_tables():
    n_freq = 32
    inv = (10000.0 ** (-np.arange(n_freq, dtype=np.float32) / n_freq)).astype(np.float32)
    t = np.arange(8192)
    rows = (t // 64).astype(np.float32)
    cols = (t % 64).astype(np.float32)
    ang_r = rows[:, None] * inv[None, :]
    ang_c = cols[:, None] * inv[None, :]
    cosd = np.concatenate([np.cos(ang_r), np.cos(ang_r), np.cos(ang_c), np.cos(ang_c)], axis=1).astype(np.float32)
    sind = np.concatenate([np.sin(ang_r), np.sin(ang_r), np.sin(ang_c), np.sin(ang_c)], axis=1).astype(np.float32)
    return cosd, sind


def rot_matrix():
    Rm = np.zeros((128, 128), np.float32)
    for blk in (0, 64):
        for d in range(32):
            Rm[blk + d + 32, blk + d] = -1.0
            Rm[blk + d, blk + d + 32] = 1.0
    return Rm


def stage_mod(inp):
    cc = np.stack([fm_vec(inp["c"][0]), fm_vec(inp["c_ctx"])], axis=-1)
    maps = []
    for r in range(NCORES):
        m = {"cc": cc}
        bm = np.zeros((2, 2, 3072), np.float32)
        for l in range(2):
            w = inp["l%d_w_mod" % l][:, r * 3072:(r + 1) * 3072]
            m["wm%d" % l] = np.ascontiguousarray(w.reshape(NCH, 128, 3072))
            bm[l, :, :] = inp["l%d_b_mod" % l][r * 3072:(r + 1) * 3072][None, :]
        m["bm"] = bm
        maps.append(m)
    res = run("mod", maps)
    full = np.concatenate([res[r]["modout"] for r in range(NCORES)], axis=-1)
    modv = np.ascontiguousarray(full.reshape(2, 2, 192, 128).transpose(3, 0, 1, 2))
    return modv


def consts():
    return {"ident_r": np.eye(128, dtype=np.float32), "ones_r": np.ones((128, 128), np.float32)}


def stage_l0a(inp, modv):
    x = inp["x"][0]
    xpad = np.zeros((8192 + 32, D), np.float32)
    xpad[16:16 + 8192] = x
    ctxT = np.ascontiguousarray(inp["ctx"][0].T.reshape(NCH, 128, 256).transpose(1, 0, 2))
    w = inp["l0_w_in"]
    order = list(range(0, 24 * 128))
    wt = wtiles(w)
    perm = list(range(24))
    for j in range(16):
        perm += [24 + j, 40 + j]
    wt = np.ascontiguousarray(wt[perm])
    gains = np.stack([inp["l0_q_gain"], inp["l0_k_gain"]], axis=1).astype(np.float32)
    cosd, sind = rope_tables()
    convp = np.zeros((128, 16, 34), np.float32)
    convp[:, :, 0:31] = inp["l0_dw_w"].T.reshape(16, 128, 31).transpose(1, 0, 2)
    convp[:, :, 31] = inp["l0_dw_b"].reshape(16, 128).T
    convp[:, :, 32] = inp["l0_conv_ln_g"].reshape(16, 128).T
    convp[:, :, 33] = inp["l0_conv_ln_b"].reshape(16, 128).T
    Rm = rot_matrix()
    maps = []
    for r in range(NCORES):
        xe = xpad[r * T:r * T + EXT]
        xT = np.ascontiguousarray(xe.T.reshape(NCH, 128, EXT).transpose(1, 0, 2))
        hm = np.ones((1, EXT), np.float32)
        gt = r * T - 16 + np.arange(EXT)
        hm[0, (gt < 0) | (gt >= 8192)] = 0.0
        m = dict(consts())
        m.update({"modv": modv, "xT": xT, "hmask": hm, "ctxT": ctxT, "w_in0": wt, "gains": gains,
                  "cosT": np.ascontiguousarray(cosd[r * T:(r + 1) * T].T), "sinT": np.ascontiguousarray(sind[r * T:(r + 1) * T].T),
                  "Rm": Rm, "convp": convp})
        maps.append(m)
    return run("l0a", maps), maps


def stage_rest(inp, modv, res_a, maps_a):
    KTc = np.asarray(res_a[0]["KTc"])
    Vc = np.asarray(res_a[0]["Vc"])
    KTa = np.concatenate([KTc] + [np.asarray(res_a[r]["KTl"]) for r in range(NCORES)], axis=2)
    Vall = np.concatenate([Vc] + [np.asarray(res_a[r]["Vl"]) for r in range(NCORES)], axis=1)
    Va = np.ascontiguousarray(Vall.reshape(4, 66, 128, 128).transpose(0, 2, 1, 3))
    lnp = np.stack([np.stack([fm_vec(inp[p + "_ln_g"]), fm_vec(inp[p + "_ln_b"])], axis=1)
                    for p in ("l0_mix", "l0_ffn", "l1_mix", "l1_ffn")], axis=0)
    shared = dict(consts())
    shared.update({"modv": modv, "KTa": KTa, "Va": Va, "lnp": np.ascontiguousarray(lnp)})
    for l in range(2):
        p = "l%d_" % l
        shared["w_out%d" % l] = wtiles(inp[p + "w_out"])
        shared["wq%d" % l] = wtiles(inp[p + "peer_wq"])
        shared["k1T%d" % l] = np.ascontiguousarray(inp[p + "peer_k1"].transpose(2, 0, 1))
        shared["k2T%d" % l] = np.ascontiguousarray(inp[p + "peer_k2"].transpose(2, 0, 1))
        shared["UT%d" % l] = wtiles(inp[p + "peer_u"].T)
        shared["VR%d" % l] = np.ascontiguousarray(inp[p + "peer_v"].reshape(128, 128, 8, 512).transpose(2, 0, 1, 3))
    w1 = inp["l1_w_in"]
    shared["w1u"] = wtiles(w1[:, :4096])
    shared["w1v"] = np.ascontiguousarray(w1[:, 4096:].reshape(NCH, 128, 8, 512).transpose(2, 1, 0, 3))
    shared["sgu_gb"] = np.stack([inp["l1_sgu_ln_g"], inp["l1_sgu_ln_b"]], axis=0).astype(np.float32)
    shared["sguWT"] = np.ascontiguousarray(inp["l1_sgu_w"].transpose(2, 0, 1))
    shared["sgub2"] = np.ascontiguousarray(np.repeat(inp["l1_sgu_b"], 2, axis=0).reshape(1, 32 * 128))
    maps = []
    for r in range(NCORES):
        m = dict(shared)
        m["xT"] = maps_a[r]["xT"]
        m["QT"] = np.asarray(res_a[r]["QT"])
        m["featc"] = np.asarray(res_a[r]["featc"])
        maps.append(m)
    res = run("rest", maps)
    out = np.empty((1, 8192, D), np.float32)
    for r in range(NCORES):
        o = np.asarray(res[r]["outT"])
        out[0, r * T:(r + 1) * T] = o.transpose(2, 0, 1).reshape(T, D)
    return out


def stage_all(inp):
    L = [
        dict(w_mod=inp["l0_w_mod"], b_mod=inp["l0_b_mod"], w_out=inp["l0_w_out"], mix_g=inp["l0_mix_ln_g"], mix_b=inp["l0_mix_ln_b"],
             wq=inp["l0_peer_wq"], k1=inp["l0_peer_k1"], k2=inp["l0_peer_k2"], u=inp["l0_peer_u"], v=inp["l0_peer_v"],
             ffn_g=inp["l0_ffn_ln_g"], ffn_b=inp["l0_ffn_ln_b"]),
        dict(w_mod=inp["l1_w_mod"], b_mod=inp["l1_b_mod"], w_out=inp["l1_w_out"], mix_g=inp["l1_mix_ln_g"], mix_b=inp["l1_mix_ln_b"],
             wq=inp["l1_peer_wq"], k1=inp["l1_peer_k1"], k2=inp["l1_peer_k2"], u=inp["l1_peer_u"], v=inp["l1_peer_v"],
             ffn_g=inp["l1_ffn_ln_g"], ffn_b=inp["l1_ffn_ln_b"]),
    ]
    x = inp["x"][0]
    sh = dict(consts())
    sh["cc"] = np.stack([fm_vec(inp["c"][0]), fm_vec(inp["c_ctx"])], axis=-1)
    sh["bmT"] = np.ascontiguousarray(np.stack([fm_vec(L[0]["b_mod"]), fm_vec(L[1]["b_mod"])], axis=1))
    lnp = []
    for l in range(2):
        sh["wmT%d" % l] = wtiles(L[l]["w_mod"])
        sh["w_out%d" % l] = wtiles(L[l]["w_out"])
        sh["wq%d" % l] = wtiles(L[l]["wq"])
        sh["k1T%d" % l] = np.ascontiguousarray(L[l]["k1"].transpose(2, 0, 1))
        sh["k2T%d" % l] = np.ascontiguousarray(L[l]["k2"].transpose(2, 0, 1))
        sh["UT%d" % l] = wtiles(L[l]["u"].T)
        sh["VR%d" % l] = np.ascontiguousarray(L[l]["v"].reshape(128, 128, 8, 512).transpose(2, 0, 1, 3))
        lnp.append(np.stack([fm_vec(L[l]["mix_g"]), fm_vec(L[l]["mix_b"])], axis=1))
        lnp.append(np.stack([fm_vec(L[l]["ffn_g"]), fm_vec(L[l]["ffn_b"])], axis=1))
    sh["lnp"] = np.ascontiguousarray(np.stack(lnp, axis=0))
    w0 = inp["l0_w_in"]
    wt = wtiles(w0)
    perm = list(range(24))
    for j in range(16):
        perm += [24 + j, 40 + j]
    sh["w_in0"] = np.ascontiguousarray(wt[perm])
    sh["w_v0"] = np.ascontiguousarray(w0[:, 2560:3072].reshape(NCH, 128, 512).transpose(1, 0, 2))
    sh["gains"] = np.stack([inp["l0_q_gain"], inp["l0_k_gain"]], axis=1).astype(np.float32)
    cosd, sind = rope_tables()
    sh["cosA"] = np.ascontiguousarray(cosd.T)
    sh["sinA"] = np.ascontiguousarray(sind.T)
    sh["Rm"] = rot_matrix()
    convp = np.zeros((128, 16, 34), np.float32)
    convp[:, :, 0:31] = inp["l0_dw_w"].T.reshape(16, 128, 31).transpose(1, 0, 2)
    convp[:, :, 31] = inp["l0_dw_b"].reshape(16, 128).T
    convp[:, :, 32] = inp["l0_conv_ln_g"].reshape(16, 128).T
    convp[:, :, 33] = inp["l0_conv_ln_b"].reshape(16, 128).T
    sh["convp"] = convp
    sh["ctxT"] = np.ascontiguousarray(inp["ctx"][0].T.reshape(NCH, 128, 256).transpose(1, 0, 2))
    sh["xTa"] = np.ascontiguousarray(x.T.reshape(NCH, 128, 8192).transpose(1, 0, 2))
    w1 = inp["l1_w_in"]
    sh["w1u"] = wtiles(w1[:, :4096])
    sh["w1v"] = np.ascontiguousarray(w1[:, 4096:].reshape(NCH, 128, 8, 512).transpose(2, 1, 0, 3))
    sh["sgu_gb"] = np.stack([inp["l1_sgu_ln_g"], inp["l1_sgu_ln_b"]], axis=0).astype(np.float32)
    sh["sguWT"] = np.ascontiguousarray(inp["l1_sgu_w"].transpose(2, 0, 1))
    sh["sgub2"] = np.ascontiguousarray(np.repeat(inp["l1_sgu_b"], 2, axis=0).reshape(1, 32 * 128))
    xpad = np.zeros((8192 + 32, D), np.float32)
    xpad[16:16 + 8192] = x
    maps = []
    for r in range(NCORES):
        m = dict(sh)
        xe = xpad[r * T:r * T + EXT]
        m["xT"] = np.ascontiguousarray(xe.T.reshape(NCH, 128, EXT).transpose(1, 0, 2))
        hm = np.ones((1, EXT), np.float32)
        gt = r * T - 16 + np.arange(EXT)
        hm[0, (gt < 0) | (gt >= 8192)] = 0.0
        m["hmask"] = hm
        m["cosT"] = np.ascontiguousarray(cosd[r * T:(r + 1) * T].T)
        m["sinT"] = np.ascontiguousarray(sind[r * T:(r + 1) * T].T)
        maps.append(m)
    res = run("all", maps)
    out = np.empty((1, 8192, D), np.float32)
    for r in range(NCORES):
        o = np.asarray(res[r]["outT"])
        out[0, r * T:(r + 1) * T] = o.transpose(2, 0, 1).reshape(T, D)
    return out


def kernel(**inp):
    inp = {k: np.asarray(v) for k, v in inp.items()}
    return stage_all(inp)
```

```python
import numpy as np
from contextlib import ExitStack
import ml_dtypes
import concourse.bass as bass
import concourse.mybir as mybir
from concourse.bass_utils import run_bass_kernel_spmd

F32 = mybir.dt.float32
F32R = mybir.dt.float32r
BF16 = mybir.dt.bfloat16
AF = mybir.ActivationFunctionType
ALU = mybir.AluOpType

NCORES = 8
T = 1024
EXT = 1056
D = 4096
NCH = 32
EPS = 1e-6
ALPHA = 4.0 ** 0.25
SAME_ENGINE_SYNC = True
SEM_ROT = 30000
NEG = -1.0e30
GELU = AF.Gelu_apprx_tanh


class Sem:
    def __init__(self, h, dma=False):
        self.h = h
        self.total = 0
        self.dma = dma


class Dep:
    __slots__ = ("w", "r", "dsem")

    def __init__(self):
        self.w = {}
        self.r = {}
        self.dsem = None


def deps(n):
    return [Dep() for _ in range(n)]


class Ctx:
    def __init__(self, nc, n_dma_sems=84):
        self.nc = nc
        self.es = ExitStack()
        self.eng = {"pe": nc.tensor, "act": nc.scalar, "dve": nc.vector, "pool": nc.gpsimd, "sp": nc.sync}
        self.esem = {}
        self.nsem = 0
        for e in self.eng:
            self.esem[e] = self._newsem(e)
        self.known = {e: {} for e in self.eng}
        self.dfree = [self._newsem("d%d" % i, dma=True) for i in range(n_dma_sems)]
        self.phase_sems = []
        self.uid = 0

    def _newsem(self, name, dma=False):
        self.nsem += 1
        h = self.es.enter_context(self.nc.semaphore("s_%s_%d" % (name, self.nsem)))
        return Sem(h, dma)

    def sbuf(self, name, shape, dt, stack=None):
        self.uid += 1
        return (stack or self.es).enter_context(self.nc.sbuf_tensor("%s_%d" % (name, self.uid), list(shape), dt))

    def psum(self, name, shape, dt=F32, stack=None):
        self.uid += 1
        return (stack or self.es).enter_context(self.nc.psum_tensor("%s_%d" % (name, self.uid), list(shape), dt))

    def dsem_for(self, dep):
        if dep.dsem is None:
            if not self.dfree:
                raise RuntimeError("out of dma sems")
            dep.dsem = self.dfree.pop(0)
            self.phase_sems.append(dep)
        return dep.dsem

    def _collect(self, reads, writes, pwrites=()):
        waits = {}

        def upd(dd):
            for s, v in dd.items():
                if waits.get(s, 0) < v:
                    waits[s] = v
        for d in reads:
            upd(d.w)
        for d in writes:
            upd(d.w)
            upd(d.r)
        for d in pwrites:
            upd(d.r)
        return waits

    def _emit_waits(self, e, waits):
        eng = self.eng[e]
        kn = self.known[e]
        own = self.esem[e]
        for s, v in waits.items():
            if s.dma:
                v = s.total
            if s is own and (e == "pe" or not SAME_ENGINE_SYNC):
                continue
            if kn.get(s, 0) >= v:
                continue
            eng.wait_ge(s.h, v)
            kn[s] = v

    def _record(self, s, tok, reads, writes, pwrites):
        for d in reads:
            if d.r.get(s, 0) < tok:
                d.r[s] = tok
        for d in writes:
            d.w = {s: tok}
            d.r = {}
        for d in pwrites:
            d.w[s] = tok

    def op(self, e, fn, reads=(), writes=(), pwrites=(), inc=True):
        self._emit_waits(e, self._collect(reads, writes, pwrites))
        ins = fn(self.eng[e])
        s = self.esem[e]
        if inc:
            if s.total >= SEM_ROT:
                s = self.esem[e] = self._newsem(e)
            s.total += 1
            ins.then_inc(s.h, 1)
            tok = s.total
        else:
            tok = s.total + 1
        self._record(s, tok, reads, writes, pwrites)
        return ins

    def dma(self, q, out, in_, reads=(), writes=(), pwrites=(), owner=None, **kw):
        self._emit_waits(q, self._collect(reads, writes, pwrites))
        if owner is None:
            owner = writes[0] if writes else (pwrites[0] if pwrites else reads[0])
        s = self.dsem_for(owner)
        ins = self.eng[q].dma_start(out=out, in_=in_, **kw)
        s.total += 16
        ins.then_inc(s.h, 16)
        self._record(s, s.total, reads, writes, pwrites)
        return ins

    def barrier(self):
        allw = {}
        for e, s in self.esem.items():
            if s.total > 0:
                allw[s] = s.total
        for d in self.phase_sems:
            if d.dsem.total > 0:
                allw[d.dsem] = d.dsem.total
        for e in self.eng:
            self._emit_waits(e, {s: v for s, v in allw.items() if s is not self.esem[e]})
        for d in self.phase_sems:
            self.dfree.append(d.dsem)
            d.dsem = None
        self.phase_sems = []

    def finish(self):
        self.barrier()
        self.es.close()


class Prog:
    def __init__(self, stage):
        self.stage = stage
        nc = bass.Bass("TRN2", target_bir_lowering=False)
        nc.dge_precook = False
        self.nc = nc
        self.c = Ctx(nc)
        self.ddeps = {}
        self.dram = {}

    def din(self, name, shape, dt):
        ap = self.nc.dram_tensor(name, list(shape), dt, kind="ExternalInput").ap()
        self.dram[name] = ap
        self.ddeps[name] = Dep()
        return ap

    def dout(self, name, shape, dt):
        ap = self.nc.dram_tensor(name, list(shape), dt, kind="ExternalOutput").ap()
        self.dram[name] = ap
        self.ddeps[name] = Dep()
        return ap

    def dint(self, name, shape, dt):
        ap = self.nc.dram_tensor(name, list(shape), dt, kind="Internal").ap()
        self.dram[name] = ap
        self.ddeps[name] = Dep()
        return ap

    def load(self, q, out, name, in_ap, dep, partial=False, **kw):
        dd = self.ddeps[name]
        if partial:
            return self.c.dma(q, out, in_ap, reads=[dd], pwrites=[dep], owner=dep, **kw)
        return self.c.dma(q, out, in_ap, reads=[dd], writes=[dep], owner=dep, **kw)

    def store(self, q, name, out_ap, in_, dep, **kw):
        dd = self.ddeps[name]
        return self.c.dma(q, out_ap, in_, reads=[dep], pwrites=[dd], owner=dep, **kw)


def cast_dma_kw():
    return dict(max_dma_last_dim=4096)


def gemm_fm(P, ph, pfx, wname, wd, n_ct, rhs, rhs_deps, nchunks, epi, nk=NCH, ct_order=None):
    c = P.c
    nslot = 3
    with ExitStack() as gs:
        ws = [c.sbuf(pfx + "_w", [128, nk, 128], BF16, gs) for _ in range(nslot)]
        wdp = deps(nslot)
        npb = len(nchunks)
        nbuf = 2 if 2 * npb <= 6 else 1
        ps = [[c.psum(pfx + "_p", [128, 512], F32, gs) for _ in range(npb)] for _ in range(nbuf)]
        psd = [deps(npb) for _ in range(nbuf)]
        order = list(range(n_ct)) if ct_order is None else ct_order
        pend = None
        for it, ct in enumerate(order):
            s = it % nslot
            P.load("pool", ws[s][:], wname, wd[ct], wdp[s], **cast_dma_kw())
            b = it % nbuf
            for j, (c0, n) in enumerate(nchunks):
                for k in range(nk):
                    c.op("pe", lambda e: e.matmul(ps[b][j][:, :n], ws[s][:, k, :], rhs[:, k, c0:c0 + n],
                                                  start=(k == 0), stop=(k == nk - 1)),
                         reads=[wdp[s], rhs_deps[k]], writes=[psd[b][j]], inc=(k == nk - 1))
                if pend is not None:
                    epi(*pend)
                pend = (ct, j, ps[b][j], psd[b][j], n)
        if pend is not None:
            epi(*pend)
        c.barrier()


def ln_phase(P, pfx, uname, gb_name, xout_name, hT=None, hT_deps=None, mod_sb=None, lidx=0, mshift=0, mscale=1):
    c = P.c
    u_d = P.dram[uname]
    x_d = P.dram[xout_name]
    with ExitStack() as ph:
        ones = c.sbuf(pfx + "ones", [128, 128], F32R, ph)
        d_ones = Dep()
        P.load("sp", ones[:], "ones_r", P.dram["ones_r"], d_ones)
        gb = c.sbuf(pfx + "gb", [128, 2, NCH], F32, ph)
        d_gb = Dep()
        P.load("sp", gb[:], gb_name, P.dram[gb_name], d_gb)
        ub = [c.sbuf(pfx + "u", [128, T], F32R, ph) for _ in range(3)]
        ubd = deps(3)
        sq = [c.sbuf(pfx + "sq", [128, T], F32R, ph) for _ in range(2)]
        sqd = deps(2)
        pss = [c.psum(pfx + "ps", [128, 512], F32, ph) for _ in range(4)]
        pssd = deps(4)
        for ch in range(NCH):
            s = ch % 3
            P.load("sp", ub[s][:], uname, u_d[ch], ubd[s])
            q = ch % 2
            c.op("act", lambda e: e.activation(out=sq[q][:], in_=ub[s][:], func=AF.Square), reads=[ubd[s]], writes=[sqd[q]])
            for half in range(2):
                sl = slice(half * 512, half * 512 + 512)
                c.op("pe", lambda e: e.matmul(pss[half][:], ones[:], ub[s][:, sl], start=(ch == 0), stop=(ch == NCH - 1)),
                     reads=[d_ones, ubd[s]], writes=[pssd[half]], inc=True)
                c.op("pe", lambda e: e.matmul(pss[2 + half][:], ones[:], sq[q][:, sl], start=(ch == 0), stop=(ch == NCH - 1)),
                     reads=[d_ones, sqd[q]], writes=[pssd[2 + half]], inc=True)
        mean = c.sbuf(pfx + "mean", [128, T], F32, ph)
        rstd = c.sbuf(pfx + "rstd", [128, T], F32, ph)
        nmr = c.sbuf(pfx + "nmr", [128, T], F32, ph)
        tmp = c.sbuf(pfx + "tmp", [128, T], F32, ph)
        d_mean, d_rstd, d_nmr, d_tmp = deps(4)
        for half in range(2):
            sl = slice(half * 512, half * 512 + 512)
            c.op("act", lambda e: e.mul(mean[:, sl], pss[half][:], 1.0 / D), reads=[pssd[half]], pwrites=[d_mean])
            c.op("dve", lambda e: e.tensor_tensor(out=tmp[:, sl], in0=mean[:, sl], in1=mean[:, sl], op=ALU.mult), reads=[d_mean], pwrites=[d_tmp])
            c.op("dve", lambda e: e.scalar_tensor_tensor(out=rstd[:, sl], in0=pss[2 + half][:], scalar=1.0 / D, in1=tmp[:, sl],
                                                         op0=ALU.mult, op1=ALU.subtract), reads=[pssd[2 + half], d_tmp], pwrites=[d_rstd])
            c.op("dve", lambda e: e.tensor_scalar(out=rstd[:, sl], in0=rstd[:, sl], scalar1=EPS, scalar2=None, op0=ALU.add), reads=[d_rstd], pwrites=[d_rstd])
            c.op("act", lambda e: e.activation(out=rstd[:, sl], in_=rstd[:, sl], func=AF.Sqrt), reads=[d_rstd], pwrites=[d_rstd])
            c.op("dve", lambda e: e.reciprocal(out=rstd[:, sl], in_=rstd[:, sl]), reads=[d_rstd], pwrites=[d_rstd])
            c.op("dve", lambda e: e.scalar_tensor_tensor(out=nmr[:, sl], in0=mean[:, sl], scalar=-1.0, in1=rstd[:, sl],
                                                         op0=ALU.mult, op1=ALU.mult), reads=[d_mean, d_rstd], pwrites=[d_nmr])
        if hT is not None:
            sc1 = c.sbuf(pfx + "sc1", [128, NCH], F32, ph)
            d_sc1 = Dep()
            c.op("dve", lambda e: e.tensor_scalar(out=sc1[:], in0=mod_sb[:, lidx, 0, mscale * NCH:(mscale + 1) * NCH], scalar1=1.0, scalar2=None, op0=ALU.add),
                 reads=[P.d_mod], writes=[d_sc1])
        xn = [c.sbuf(pfx + "xn", [128, T], F32, ph) for _ in range(2)]
        xnd = deps(2)
        for ch in range(NCH):
            s = ch % 3
            q = ch % 2
            P.load("sp", ub[s][:], uname, u_d[ch], ubd[s])
            uf = ub[s][:].bitcast(F32)
            c.op("dve", lambda e: e.tensor_tensor(out=xn[q][:], in0=uf, in1=rstd[:], op=ALU.mult), reads=[ubd[s], d_rstd], writes=[xnd[q]])
            c.op("pool", lambda e: e.tensor_tensor(out=xn[q][:], in0=xn[q][:], in1=nmr[:], op=ALU.add), reads=[xnd[q], d_nmr], writes=[xnd[q]])
            c.op("dve", lambda e: e.tensor_scalar(out=xn[q][:], in0=xn[q][:], scalar1=gb[:, 0, ch:ch + 1], scalar2=gb[:, 1, ch:ch + 1],
                                                  op0=ALU.mult, op1=ALU.add), reads=[xnd[q], d_gb], writes=[xnd[q]])
            P.store("sp", xout_name, x_d[ch], xn[q][:], xnd[q])
            if hT is not None:
                c.op("act", lambda e: e.activation(out=hT[:, ch, :], in_=xn[q][:], func=AF.Identity,
                                                   scale=sc1[:, ch:ch + 1], bias=mod_sb[:, lidx, 0, mshift * NCH + ch:mshift * NCH + ch + 1]),
                     reads=[xnd[q], d_sc1, P.d_mod], writes=[hT_deps[ch]])
        c.barrier()


def build(stage):
    P = Prog(stage)
    c = P.c
    nc = P.nc
    if stage == "mod":
        cc = P.din("cc", [128, NCH, 2], F32)
        wm = [P.din("wm%d" % l, [NCH, 128, 3072], F32) for l in range(2)]
        bm = P.din("bm", [2, 2, 3072], F32)
        mo = P.dout("modout", [2, 2, 3072], F32)
        with ExitStack() as ph:
            cs = c.sbuf("cs", [128, NCH, 2], F32, ph)
            d_cs = Dep()
            P.load("sp", cs[:], "cc", cc, d_cs)
            c.op("act", lambda e: e.activation(out=cs[:], in_=cs[:], func=AF.Silu), reads=[d_cs], writes=[d_cs])
            bs = c.sbuf("bs", [2, 2, 3072], F32, ph)
            d_bs = Dep()
            P.load("sp", bs[:], "bm", bm.rearrange("l r n -> r l n"), d_bs)
            res = c.sbuf("res", [2, 2, 3072], F32, ph)
            d_res = Dep()
            ws = [c.sbuf("wms", [128, 3072], F32, ph) for _ in range(3)]
            wsd = deps(3)
            ps = [c.psum("mps", [128, 512], F32, ph) for _ in range(6)]
            psd = deps(6)
            for l in range(2):
                for k in range(NCH):
                    s = k % 3
                    P.load("sp", ws[s][:], "wm%d" % l, wm[l][k], wsd[s])
                    for j in range(6):
                        c.op("pe", lambda e: e.matmul(ps[j][0:2, :], cs[:, k, :], ws[s][:, j * 512:(j + 1) * 512],
                                                      start=(k == 0), stop=(k == NCH - 1)),
                             reads=[d_cs, wsd[s]], writes=[psd[j]], inc=True)
                for j in range(6):
                    c.op("dve", lambda e: e.tensor_tensor(out=res[:, l, j * 512:(j + 1) * 512], in0=ps[j][0:2, :],
                                                          in1=bs[:, l, j * 512:(j + 1) * 512], op=ALU.add),
                         reads=[psd[j], d_bs], pwrites=[d_res])
            P.store("sp", "modout", mo.rearrange("l r n -> r l n"), res[:], d_res)
            c.barrier()
        c.finish()
        return nc

    FUSED = stage == "all"
    ident_r = P.din("ident_r", [128, 128], F32R)
    ones_r = P.din("ones_r", [128, 128], F32R)
    mod_sb = c.sbuf("mod_sb", [128, 2, 2, 192], F32)
    P.d_mod = Dep()
    if not FUSED:
        modv = P.din("modv", [128, 2, 2, 192], F32)
        P.load("sp", mod_sb[:], "modv", modv, P.d_mod)
    else:
        ccr = P.din("cc", [128, NCH, 2], F32)
        wmT = [P.din("wmT%d" % l, [192, 128, NCH, 128], F32R) for l in range(2)]
        bmT = P.din("bmT", [128, 2, 192], F32)
        with ExitStack() as ph:
            cs0 = c.sbuf("mcs0", [128, NCH, 2], F32, ph); d_cs0 = Dep()
            cs = c.sbuf("mcs", [128, NCH, 2], F32R, ph); d_cs = Dep()
            P.load("sp", cs0[:], "cc", ccr, d_cs0)
            c.op("act", lambda e: e.activation(out=cs[:], in_=cs0[:], func=AF.Silu), reads=[d_cs0], writes=[d_cs])
            bs = c.sbuf("mbs", [128, 2, 192], F32, ph); d_bs = Dep()
            P.load("sp", bs[:], "bmT", bmT, d_bs)
            wsl = [c.sbuf("mws", [128, NCH, 128], F32R, ph) for _ in range(4)]; wsd = deps(4)
            mps = [c.psum("mps", [128, 512], F32, ph) for _ in range(2)]; mpd = deps(2)
            nq = 0
            for l in range(2):
                for j in range(192):
                    s = nq % 4; nq += 1
                    P.load("sp" if nq % 2 else "act", wsl[s][:], "wmT%d" % l, wmT[l][j], wsd[s])
                    for k in range(NCH):
                        c.op("pe", lambda e: e.matmul(mps[l][:, 2 * j:2 * j + 2], wsl[s][:, k, :], cs[:, k, :], start=(k == 0), stop=(k == NCH - 1)),
                             reads=[wsd[s], d_cs], writes=[mpd[l]], inc=(k == NCH - 1))
                c.op("dve", lambda e: e.tensor_tensor(out=mod_sb[:, l, :, :], in0=mps[l][:, 0:384].rearrange("p (j r) -> p r j", r=2),
                                                      in1=bs[:, l, :].unsqueeze(1).to_broadcast([128, 2, 192]), op=ALU.add),
                     reads=[mpd[l], d_bs], pwrites=[P.d_mod])
            c.barrier()

    if stage in ("l0a", "all"):
        mk = P.dint if FUSED else P.dout
        xT = P.din("xT", [128, NCH, EXT], F32)
        hmask = P.din("hmask", [1, EXT], F32)
        ctxT = P.din("ctxT", [128, NCH, 256], F32)
        w_in0 = P.din("w_in0", [56, 128, NCH, 128], F32)
        gains = P.din("gains", [128, 2], F32)
        cosT = P.din("cosT", [128, T], F32)
        sinT = P.din("sinT", [128, T], F32)
        Rm = P.din("Rm", [128, 128], F32R)
        convp = P.din("convp", [128, 16, 34], F32)
        QT = mk("QT", [16, 128, T], BF16)
        KTl = mk("KTl", [4, 128, T], BF16)
        Vl = mk("Vl", [4, T, 128], BF16)
        KTc = mk("KTc", [4, 128, 256], BF16)
        Vc = mk("Vc", [4, 256, 128], BF16)
        featc = mk("featc", [16, 128, T], BF16)
        Gd = P.dint("Gd", [16, 128, EXT], F32R)
        if FUSED:
            xTa = P.din("xTa", [128, NCH, 8192], F32)
            cosA = P.din("cosA", [128, 8192], F32)
            sinA = P.din("sinA", [128, 8192], F32)
            w_v0 = P.din("w_v0", [128, NCH, 512], F32)
            KTa = P.dint("KTa", [4, 128, 8448], BF16)
            Va = P.dint("Va", [4, 128, 66, 128], BF16)
            with ExitStack() as ph:
                wk = c.sbuf("kvwk", [128, 4, NCH, 128], BF16, ph); d_wk = Dep()
                wv = c.sbuf("kvwv", [128, NCH, 512], BF16, ph); d_wv = Dep()
                for g in range(4):
                    P.load("pool", wk[:, g], "w_in0", w_in0[16 + g], d_wk, partial=True, **cast_dma_kw())
                P.load("pool", wv[:], "w_v0", w_v0, d_wv, **cast_dma_kw())
                hb = [c.sbuf("kvh", [128, NCH, 512], BF16, ph) for _ in range(2)]; hbd = [deps(NCH) for _ in range(2)]
                sc1 = c.sbuf("kvsc1", [128, 2, NCH], F32, ph); d_sc1 = Dep()
                c.op("dve", lambda e: e.tensor_scalar(out=sc1[:, 0, :], in0=mod_sb[:, 0, 0, 32:64], scalar1=1.0, scalar2=None, op0=ALU.add), reads=[P.d_mod], pwrites=[d_sc1])
                c.op("dve", lambda e: e.tensor_scalar(out=sc1[:, 1, :], in0=mod_sb[:, 0, 1, 32:64], scalar1=1.0, scalar2=None, op0=ALU.add), reads=[P.d_mod], pwrites=[d_sc1])
                xs = [c.sbuf("kvxs", [128, 512], F32, ph) for _ in range(6)]; xsd = deps(6)
                csn = [c.sbuf("kvcs", [128, 2, 512], F32, ph) for _ in range(2)]; csnd = deps(2)
                gn = c.sbuf("kvgn", [128, 2], F32, ph); d_gn = Dep()
                P.load("sp", gn[:], "gains", gains, d_gn)
                rm_sb = c.sbuf("kvrm", [128, 128], F32R, ph); d_rm = Dep()
                P.load("sp", rm_sb[:], "Rm", Rm, d_rm)
                on_sb = c.sbuf("kvones", [128, 128], F32R, ph); d_on = Dep()
                P.load("sp", on_sb[:], "ones_r", ones_r, d_on)
                sq = [c.sbuf("kvsq", [128, 512], F32R, ph) for _ in range(2)]; sqd = deps(2)
                rin = [c.sbuf("kvrin", [128, 512], F32, ph) for _ in range(2)]; rind = deps(2)
                qn = [c.sbuf("kvqn", [128, 512], F32R, ph) for _ in range(2)]; qnd = deps(2)
                t1 = [c.sbuf("kvt1", [128, 512], F32, ph) for _ in range(2)]; t1d = deps(2)
                t2 = [c.sbuf("kvt2", [128, 512], F32, ph) for _ in range(2)]; t2d = deps(2)
                ko = [c.sbuf("kvko", [128, 512], BF16, ph) for _ in range(3)]; kod = deps(3)
                vo2 = [c.sbuf("kvvo", [128, 4, 128], BF16, ph) for _ in range(3)]; vo2d = deps(3)
                pk = [c.psum("kvpk", [128, 512], F32, ph) for _ in range(4)]; pkd = deps(4)
                pss = c.psum("kvps", [128, 512], F32, ph); pssd = Dep()
                psr = c.psum("kvpr", [128, 512], F32, ph); psrd = Dep()
                pv2 = [c.psum("kvpv", [128, 512], F32, ph) for _ in range(2)]; pv2d = deps(2)
                nx = [0]; nk_ = 0; nv_ = 0

                def modulate(blk):
                    b = blk % 2
                    n = 512 if blk < 16 else 256
                    row = 0 if blk < 16 else 1
                    for ch in range(NCH):
                        s4 = nx[0] % 6; nx[0] += 1
                        src = xTa[:, ch, blk * 512:(blk + 1) * 512] if blk < 16 else ctxT[:, ch, :]
                        P.load("sp", xs[s4][:, :n], "xTa" if blk < 16 else "ctxT", src, xsd[s4])
                        if ch % 2:
                            c.op("dve", lambda e: e.tensor_scalar(out=hb[b][:, ch, :n], in0=xs[s4][:, :n], scalar1=sc1[:, row, ch:ch + 1],
                                                                  scalar2=mod_sb[:, 0, row, ch:ch + 1], op0=ALU.mult, op1=ALU.add),
                                 reads=[xsd[s4], d_sc1, P.d_mod], writes=[hbd[b][ch]])
                        else:
                            c.op("act", lambda e: e.activation(out=hb[b][:, ch, :n], in_=xs[s4][:, :n], func=AF.Identity,
                                                               scale=sc1[:, row, ch:ch + 1], bias=mod_sb[:, 0, row, ch:ch + 1]),
                                 reads=[xsd[s4], d_sc1, P.d_mod], writes=[hbd[b][ch]])
                    if blk < 16:
                        P.load("sp", csn[b][:, 0, :], "cosA", cosA[:, blk * 512:(blk + 1) * 512], csnd[b], partial=True)
                        P.load("sp", csn[b][:, 1, :], "sinA", sinA[:, blk * 512:(blk + 1) * 512], csnd[b], partial=True)

                modulate(0)
                for blk in range(17):
                    b = blk % 2
                    n = 512 if blk < 16 else 256
                    for g in range(4):
                        for k in range(NCH):
                            c.op("pe", lambda e: e.matmul(pk[g][:, :n], wk[:, g, k, :], hb[b][:, k, :n], start=(k == 0), stop=(k == NCH - 1)),
                                 reads=[d_wk, hbd[b][k]], writes=[pkd[g]], inc=(k == NCH - 1))
                    for g in range(4):
                        i = nk_ % 2; o3 = nk_ % 3; nk_ += 1
                        ps, psd = pk[g], pkd[g]
                        c.op("act", lambda e: e.activation(out=sq[i][:, :n], in_=ps[:, :n], func=AF.Square), reads=[psd], writes=[sqd[i]])
                        c.op("pe", lambda e: e.matmul(pss[:, :n], on_sb[:], sq[i][:, :n], start=True, stop=True), reads=[d_on, sqd[i]], writes=[pssd])
                        c.op("act", lambda e: e.mul(rin[i][:, :n], pss[:, :n], 1.0 / 128.0), reads=[pssd], writes=[rind[i]])
                        c.op("dve", lambda e: e.tensor_scalar(out=rin[i][:, :n], in0=rin[i][:, :n], scalar1=EPS, scalar2=None, op0=ALU.add), reads=[rind[i]], writes=[rind[i]])
                        c.op("act", lambda e: e.activation(out=rin[i][:, :n], in_=rin[i][:, :n], func=AF.Sqrt), reads=[rind[i]], writes=[rind[i]])
                        c.op("dve", lambda e: e.reciprocal(out=rin[i][:, :n], in_=rin[i][:, :n]), reads=[rind[i]], writes=[rind[i]])
                        if blk == 16:
                            c.op("dve", lambda e: e.scalar_tensor_tensor(out=ko[o3][:, :n], in0=ps[:, :n], scalar=gn[:, 1:2], in1=rin[i][:, :n],
                                                                         op0=ALU.mult, op1=ALU.mult), reads=[psd, d_gn, rind[i]], writes=[kod[o3]])
                            P.store("sp", "KTa", KTa[g][:, 0:256], ko[o3][:, :n], kod[o3])
                        else:
                            c.op("dve", lambda e: e.scalar_tensor_tensor(out=qn[i][:, :n], in0=ps[:, :n], scalar=gn[:, 1:2], in1=rin[i][:, :n],
                                                                         op0=ALU.mult, op1=ALU.mult), reads=[psd, d_gn, rind[i]], writes=[qnd[i]])
                            c.op("pe", lambda e: e.matmul(psr[:, :n], rm_sb[:], qn[i][:, :n], start=True, stop=True), reads=[d_rm, qnd[i]], writes=[psrd])
                            c.op("pool", lambda e: e.tensor_tensor(out=t1[i][:, :n], in0=qn[i][:, :n].bitcast(F32), in1=csn[b][:, 0, :], op=ALU.mult), reads=[qnd[i], csnd[b]], writes=[t1d[i]])
                            c.op("dve", lambda e: e.tensor_tensor(out=t2[i][:, :n], in0=psr[:, :n], in1=csn[b][:, 1, :], op=ALU.mult), reads=[psrd, csnd[b]], writes=[t2d[i]])
                            c.op("dve", lambda e: e.tensor_tensor(out=ko[o3][:, :n], in0=t1[i][:, :n], in1=t2[i][:, :n], op=ALU.add), reads=[t1d[i], t2d[i]], writes=[kod[o3]])
                            P.store("sp", "KTa", KTa[g][:, 256 + blk * 512:256 + (blk + 1) * 512], ko[o3][:, :n], kod[o3])
                    if blk + 1 < 17:
                        modulate(blk + 1)
                    for tt in range(n // 128):
                        pb = nv_ % 2; o3 = nv_ % 3; nv_ += 1
                        for k in range(NCH):
                            c.op("pe", lambda e: e.matmul(pv2[pb][:], hb[b][:, k, tt * 128:(tt + 1) * 128], wv[:, k, :], start=(k == 0), stop=(k == NCH - 1)),
                                 reads=[hbd[b][k], d_wv], writes=[pv2d[pb]], inc=(k == NCH - 1))
                        c.op("act", lambda e: e.copy(vo2[o3][:].rearrange("p g d -> p (g d)"), pv2[pb][:]), reads=[pv2d[pb]], writes=[vo2d[o3]])
                        st = (2 + blk * 4 + tt) if blk < 16 else tt
                        P.store("sp", "Va", Va[:, :, st, :].rearrange("g p d -> p g d"), vo2[o3][:], vo2d[o3])
                c.barrier()
        with ExitStack() as ph:
            hT = c.sbuf("hT", [128, NCH, EXT], BF16, ph)
            hTd = deps(NCH)
            hcT = c.sbuf("hcT", [128, NCH, 256], BF16, ph)
            hcTd = deps(NCH)
            sc1 = c.sbuf("sc1", [128, 2, NCH], F32, ph)
            d_sc1 = Dep()
            c.op("dve", lambda e: e.tensor_scalar(out=sc1[:, 0, :], in0=mod_sb[:, 0, 0, 32:64], scalar1=1.0, scalar2=None, op0=ALU.add),
                 reads=[P.d_mod], pwrites=[d_sc1])
            c.op("dve", lambda e: e.tensor_scalar(out=sc1[:, 1, :], in0=mod_sb[:, 0, 1, 32:64], scalar1=1.0, scalar2=None, op0=ALU.add),
                 reads=[P.d_mod], pwrites=[d_sc1])
            xs = [c.sbuf("xs", [128, EXT], F32, ph) for _ in range(3)]
            xsd = deps(3)
            cxs = [c.sbuf("cxs", [128, 256], F32, ph) for _ in range(2)]
            cxd = deps(2)
            for ch in range(NCH):
                s = ch % 3
                P.load("sp", xs[s][:], "xT", xT[:, ch, :], xsd[s])
                c.op("dve", lambda e: e.tensor_scalar(out=hT[:, ch, :], in0=xs[s][:], scalar1=sc1[:, 0, ch:ch + 1],
                                                      scalar2=mod_sb[:, 0, 0, ch:ch + 1], op0=ALU.mult, op1=ALU.add),
                     reads=[xsd[s], d_sc1, P.d_mod], writes=[hTd[ch]])
                s2 = ch % 2
                if FUSED:
                    continue
                P.load("sp", cxs[s2][:], "ctxT", ctxT[:, ch, :], cxd[s2])
                c.op("pool", lambda e: e.tensor_scalar(out=hcT[:, ch, :], in0=cxs[s2][:], scalar1=sc1[:, 1, ch:ch + 1],
                                                       scalar2=mod_sb[:, 0, 1, ch:ch + 1], op0=ALU.mult, op1=ALU.add),
                     reads=[cxd[s2], d_sc1, P.d_mod], writes=[hcTd[ch]])
            gn = c.sbuf("gn", [128, 2], F32, ph); d_gn = Dep()
            P.load("sp", gn[:], "gains", gains, d_gn)
            cs_sb = c.sbuf("cos", [128, T], F32, ph); sn_sb = c.sbuf("sin", [128, T], F32, ph); d_cs = Dep()
            P.load("sp", cs_sb[:], "cosT", cosT, d_cs, partial=True)
            P.load("sp", sn_sb[:], "sinT", sinT, d_cs, partial=True)
            rm_sb = c.sbuf("rm", [128, 128], F32R, ph); d_rm = Dep()
            P.load("sp", rm_sb[:], "Rm", Rm, d_rm)
            on_sb = c.sbuf("ones", [128, 128], F32R, ph); d_on = Dep()
            P.load("sp", on_sb[:], "ones_r", ones_r, d_on)
            hm = c.sbuf("hm", [128, EXT], F32, ph); d_hm = Dep()
            P.load("sp", hm[:], "hmask", hmask.partition_broadcast(128), d_hm)
            sq = [c.sbuf("sq", [128, 512], F32R, ph) for _ in range(2)]; sqd = deps(2)
            rin = [c.sbuf("rin", [128, 512], F32, ph) for _ in range(2)]; rind = deps(2)
            qn = [c.sbuf("qn", [128, 512], F32R, ph) for _ in range(2)]; qnd = deps(2)
            t1 = [c.sbuf("t1", [128, 512], F32, ph) for _ in range(2)]; t1d = deps(2)
            t2 = [c.sbuf("t2", [128, 512], F32, ph) for _ in range(2)]; t2d = deps(2)
            qo = [c.sbuf("qo", [128, T], BF16, ph) for _ in range(2)]; qod = deps(2)
            ps_s = [c.psum("ps_s", [128, 512], F32, ph) for _ in range(1)]; ps_sd = deps(1)
            ps_r = [c.psum("ps_r", [128, 512], F32, ph) for _ in range(1)]; ps_rd = deps(1)
            vo = [c.sbuf("vo", [128, 128], BF16, ph) for _ in range(2)]; vod = deps(2)
            valb = c.sbuf("valb", [128, EXT], F32, ph); d_valb = Dep()
            sig = c.sbuf("sig", [128, EXT], F32, ph); d_sig = Dep()
            gb = [c.sbuf("gb", [128, EXT], F32R, ph) for _ in range(2)]; gbd = deps(2)
            cnt = {"n": 0}

            def normrope(ps, psd, n, gcol, rope, dst, dstd, dsl):
                i = cnt["n"] % 2
                cnt["n"] += 1
                c.op("act", lambda e: e.activation(out=sq[i][:, :n], in_=ps[:, :n], func=AF.Square), reads=[psd], writes=[sqd[i]])
                c.op("pe", lambda e: e.matmul(ps_s[0][:, :n], on_sb[:], sq[i][:, :n], start=True, stop=True), reads=[d_on, sqd[i]], writes=[ps_sd[0]])
                c.op("act", lambda e: e.mul(rin[i][:, :n], ps_s[0][:, :n], 1.0 / 128.0), reads=[ps_sd[0]], writes=[rind[i]])
                c.op("dve", lambda e: e.tensor_scalar(out=rin[i][:, :n], in0=rin[i][:, :n], scalar1=EPS, scalar2=None, op0=ALU.add), reads=[rind[i]], writes=[rind[i]])
                c.op("act", lambda e: e.activation(out=rin[i][:, :n], in_=rin[i][:, :n], func=AF.Sqrt), reads=[rind[i]], writes=[rind[i]])
                c.op("dve", lambda e: e.reciprocal(out=rin[i][:, :n], in_=rin[i][:, :n]), reads=[rind[i]], writes=[rind[i]])
                if not rope:
                    c.op("dve", lambda e: e.scalar_tensor_tensor(out=dst[:, dsl], in0=ps[:, :n], scalar=gn[:, gcol:gcol + 1], in1=rin[i][:, :n],
                                                                 op0=ALU.mult, op1=ALU.mult), reads=[psd, d_gn, rind[i]], pwrites=[dstd])
                    return
                c.op("dve", lambda e: e.scalar_tensor_tensor(out=qn[i][:, :n], in0=ps[:, :n], scalar=gn[:, gcol:gcol + 1], in1=rin[i][:, :n],
                                                             op0=ALU.mult, op1=ALU.mult), reads=[psd, d_gn, rind[i]], writes=[qnd[i]])
                c.op("pe", lambda e: e.matmul(ps_r[0][:, :n], rm_sb[:], qn[i][:, :n], start=True, stop=True), reads=[d_rm, qnd[i]], writes=[ps_rd[0]])
                c.op("pool", lambda e: e.tensor_tensor(out=t1[i][:, :n], in0=qn[i][:, :n].bitcast(F32), in1=cs_sb[:, dsl], op=ALU.mult), reads=[qnd[i], d_cs], writes=[t1d[i]])
                c.op("dve", lambda e: e.tensor_tensor(out=t2[i][:, :n], in0=ps_r[0][:, :n], in1=sn_sb[:, dsl], op=ALU.mult), reads=[ps_rd[0], d_cs], writes=[t2d[i]])
                c.op("dve", lambda e: e.tensor_tensor(out=dst[:, dsl], in0=t1[i][:, :n], in1=t2[i][:, :n], op=ALU.add), reads=[t1d[i], t2d[i]], pwrites=[dstd])

            state = {}

            def epi(ct, j, ps, psd, n):
                half_sl = slice(j * 512, j * 512 + n)
                if ct < 16:
                    i = ct % 2
                    if j == 0:
                        c.op("dve", lambda e: e.memset(qo[i][:, 0:2], 0.0), writes=[qod[i]])
                    normrope(ps, psd, n, 0, True, qo[i], qod[i], half_sl)
                    if j == 1:
                        P.store("sp", "QT", QT[ct], qo[i][:], qod[i])
                elif ct < 20:
                    g = ct - 16
                    i = ct % 2
                    if j == 0:
                        c.op("dve", lambda e: e.memset(qo[i][:, 0:2], 0.0), writes=[qod[i]])
                    if j < 2:
                        normrope(ps, psd, n, 1, True, qo[i], qod[i], half_sl)
                        if j == 1:
                            P.store("sp", "KTl", KTl[g], qo[i][:], qod[i])
                    else:
                        i2 = (ct + 1) % 2
                        c.op("dve", lambda e: e.memset(qo[i2][:, 0:2], 0.0), writes=[qod[i2]])
                        normrope(ps, psd, n, 1, False, qo[i2], qod[i2], slice(0, 256))
                        P.store("sp", "KTc", KTc[g], qo[i2][:, 0:256], qod[i2])
                else:
                    a = ct - 24
                    jj, isgate = a // 2, a % 2
                    esl = slice([0, 512, 1024][j], [0, 512, 1024][j] + n)
                    if not isgate:
                        c.op("act", lambda e: e.copy(valb[:, esl], ps[:, :n]), reads=[psd], pwrites=[d_valb])
                    else:
                        i = jj % 2
                        c.op("act", lambda e: e.activation(out=sig[:, esl], in_=ps[:, :n], func=AF.Sigmoid), reads=[psd], pwrites=[d_sig])
                        if j == 0:
                            c.op("dve", lambda e: e.memset(gb[i][:, 0:2].bitcast(F32), 0.0), writes=[gbd[i]])
                        c.op("dve", lambda e: e.tensor_tensor(out=sig[:, esl], in0=sig[:, esl], in1=valb[:, esl], op=ALU.mult), reads=[d_sig, d_valb], pwrites=[d_sig])
                        c.op("dve", lambda e: e.tensor_tensor(out=gb[i][:, esl], in0=sig[:, esl], in1=hm[:, esl], op=ALU.mult), reads=[d_sig, d_hm], pwrites=[gbd[i]])
                        if j == 2:
                            P.store("sp", "Gd", Gd[jj], gb[i][:], gbd[i])
                            c.op("act", lambda e: e.copy(valb[:, 0:2], valb[:, 0:2]), reads=[gbd[i]], writes=[d_valb])
                            c.op("act", lambda e: e.copy(sig[:, 0:2], sig[:, 0:2]), reads=[gbd[i]], writes=[d_sig])

            hT_main = hT[:, :, 16:16 + T]
            gemm_fm(P, ph, "gq", "w_in0", w_in0, 16, hT_main, hTd, [(0, 512), (512, 512)], epi)
            def epi_k(ct, j, ps, psd, n):
                epi(ct, j, ps, psd, n)
            if not FUSED:
              gemm_fm(P, ph, "gk", "w_in0", w_in0, 20, hT_main, hTd, [(0, 512), (512, 512)], epi_k, ct_order=[16, 17, 18, 19])
              gemm_fm(P, ph, "gkc", "w_in0", w_in0, 20, hcT, hcTd, [(0, 256)], lambda ct, j, ps, psd, n: epi(ct, 2, ps, psd, n), ct_order=[16, 17, 18, 19])
            vs = ExitStack()
            ws_v = [c.sbuf("wv", [128, NCH, 128], BF16, vs) for _ in range(2)]
            NVG = 0 if FUSED else 4
            wvd = deps(2)
            pv = [c.psum("pv", [128, 512], F32, vs) for _ in range(2)]
            pvd = deps(2)
            nv = 0
            for g in range(NVG):
                s = g % 2
                P.load("pool", ws_v[s][:], "w_in0", w_in0[20 + g], wvd[s], **cast_dma_kw())
                for tt in range(10):
                    b = nv % 2
                    for k in range(NCH):
                        if tt < 8:
                            lhs = hT[:, k, 16 + tt * 128:16 + (tt + 1) * 128]
                            rd = hTd[k]
                        else:
                            lhs = hcT[:, k, (tt - 8) * 128:(tt - 7) * 128]
                            rd = hcTd[k]
                        c.op("pe", lambda e: e.matmul(pv[b][:, 0:128], lhs, ws_v[s][:, k, :], start=(k == 0), stop=(k == NCH - 1)),
                             reads=[rd, wvd[s]], writes=[pvd[b]], inc=(k == NCH - 1))
                    c.op("act", lambda e: e.copy(vo[b][:], pv[b][:, 0:128]), reads=[pvd[b]], writes=[vod[b]])
                    if tt < 8:
                        P.store("sp", "Vl", Vl[g, tt * 128:(tt + 1) * 128, :], vo[b][:], vod[b])
                    else:
                        P.store("sp", "Vc", Vc[g, (tt - 8) * 128:(tt - 7) * 128, :], vo[b][:], vod[b])
                    nv += 1
            c.barrier()
            vs.close()
            gemm_fm(P, ph, "ga", "w_in0", w_in0, 56, hT, hTd, [(0, 512), (512, 512), (1024, 32)], epi, ct_order=list(range(24, 56)))
            c.barrier()
        with ExitStack() as ph:
            cp = c.sbuf("cp", [128, 16, 34], F32, ph); d_cp = Dep()
            P.load("sp", cp[:], "convp", convp, d_cp)
            idn = c.sbuf("idn", [128, 128], F32, ph); d_idn = Dep()
            P.load("sp", idn[:], "ident_r", ident_r.bitcast(F32), d_idn)
            on_sb = c.sbuf("ones", [128, 128], F32R, ph); d_on = Dep()
            P.load("sp", on_sb[:], "ones_r", ones_r, d_on)
            hcv = c.sbuf("hcv", [128, 16, T], F32R, ph); hcvd = deps(16)
            gl = [c.sbuf("gl", [128, EXT], F32R, ph) for _ in range(2)]; gld = deps(2)
            dg = [c.sbuf("dg", [128, 128], F32R, ph) for _ in range(4)]; dgd = deps(4)
            pc = [c.psum("pc", [128, 512], F32, ph) for _ in range(4)]; pcd = deps(4)
            pst = [c.psum("pst", [128, 512], F32, ph) for _ in range(4)]; pstd = deps(4)
            sqb = [c.sbuf("sqb", [128, T], F32R, ph) for _ in range(2)]; sqbd = deps(2)
            nd = 0
            for jj in range(16):
                s = jj % 2
                P.load("sp", gl[s][:], "Gd", Gd[jj], gld[s])
                for k in range(31):
                    di = nd % 4
                    nd += 1
                    c.op("dve", lambda e: e.tensor_scalar(out=dg[di][:], in0=idn[:], scalar1=cp[:, jj, k:k + 1], scalar2=None, op0=ALU.mult),
                         reads=[d_idn, d_cp], writes=[dgd[di]])
                    for half in range(2):
                        pb = (jj % 2) * 2 + half
                        c.op("pe", lambda e: e.matmul(pc[pb][:], dg[di][:], gl[s][:, 1 + k + half * 512:1 + k + half * 512 + 512],
                                                      start=(k == 0), stop=(k == 30)),
                             reads=[dgd[di], gld[s]], writes=[pcd[pb]], inc=True)
                for half in range(2):
                    pb = (jj % 2) * 2 + half
                    sl = slice(half * 512, half * 512 + 512)
                    c.op("act", lambda e: e.activation(out=hcv[:, jj, sl], in_=pc[pb][:], func=AF.Identity, bias=cp[:, jj, 31:32], scale=1.0),
                         reads=[pcd[pb], d_cp], pwrites=[hcvd[jj]])
                q = jj % 2
                c.op("act", lambda e: e.activation(out=sqb[q][:], in_=hcv[:, jj, :], func=AF.Square), reads=[hcvd[jj]], writes=[sqbd[q]])
                for half in range(2):
                    sl = slice(half * 512, half * 512 + 512)
                    c.op("pe", lambda e: e.matmul(pst[half][:], on_sb[:], hcv[:, jj, sl], start=(jj == 0), stop=(jj == 15)),
                         reads=[d_on, hcvd[jj]], writes=[pstd[half]], inc=True)
                    c.op("pe", lambda e: e.matmul(pst[2 + half][:], on_sb[:], sqb[q][:, sl], start=(jj == 0), stop=(jj == 15)),
                         reads=[d_on, sqbd[q]], writes=[pstd[2 + half]], inc=True)
            mean = c.sbuf("cmean", [128, T], F32, ph)
            rstd = c.sbuf("crstd", [128, T], F32, ph)
            nmr = c.sbuf("cnmr", [128, T], F32, ph)
            tmp = c.sbuf("ctmp", [128, T], F32, ph)
            d_mean, d_rstd, d_nmr, d_tmp = deps(4)
            CW = 2048.0
            for half in range(2):
                sl = slice(half * 512, half * 512 + 512)
                c.op("act", lambda e: e.mul(mean[:, sl], pst[half][:], 1.0 / CW), reads=[pstd[half]], pwrites=[d_mean])
                c.op("dve", lambda e: e.tensor_tensor(out=tmp[:, sl], in0=mean[:, sl], in1=mean[:, sl], op=ALU.mult), reads=[d_mean], pwrites=[d_tmp])
                c.op("dve", lambda e: e.scalar_tensor_tensor(out=rstd[:, sl], in0=pst[2 + half][:], scalar=1.0 / CW, in1=tmp[:, sl],
                                                             op0=ALU.mult, op1=ALU.subtract), reads=[pstd[2 + half], d_tmp], pwrites=[d_rstd])
                c.op("dve", lambda e: e.tensor_scalar(out=rstd[:, sl], in0=rstd[:, sl], scalar1=EPS, scalar2=None, op0=ALU.add), reads=[d_rstd], pwrites=[d_rstd])
                c.op("act", lambda e: e.activation(out=rstd[:, sl], in_=rstd[:, sl], func=AF.Sqrt), reads=[d_rstd], pwrites=[d_rstd])
                c.op("dve", lambda e: e.reciprocal(out=rstd[:, sl], in_=rstd[:, sl]), reads=[d_rstd], pwrites=[d_rstd])
                c.op("dve", lambda e: e.scalar_tensor_tensor(out=nmr[:, sl], in0=mean[:, sl], scalar=-1.0, in1=rstd[:, sl],
                                                             op0=ALU.mult, op1=ALU.mult), reads=[d_mean, d_rstd], pwrites=[d_nmr])
            xn = [c.sbuf("cxn", [128, T], F32, ph) for _ in range(2)]; xnd = deps(2)
            fo = [c.sbuf("cfo", [128, T], BF16, ph) for _ in range(2)]; fod = deps(2)
            for jj in range(16):
                q = jj % 2
                c.op("dve", lambda e: e.tensor_tensor(out=xn[q][:], in0=hcv[:, jj, :].bitcast(F32), in1=rstd[:], op=ALU.mult), reads=[hcvd[jj], d_rstd], writes=[xnd[q]])
                c.op("pool", lambda e: e.tensor_tensor(out=xn[q][:], in0=xn[q][:], in1=nmr[:], op=ALU.add), reads=[xnd[q], d_nmr], writes=[xnd[q]])
                c.op("dve", lambda e: e.tensor_scalar(out=xn[q][:], in0=xn[q][:], scalar1=cp[:, jj, 32:33], scalar2=cp[:, jj, 33:34],
                                                      op0=ALU.mult, op1=ALU.add), reads=[xnd[q], d_cp], writes=[xnd[q]])
                c.op("act", lambda e: e.activation(out=fo[q][:], in_=xn[q][:], func=AF.Silu), reads=[xnd[q]], writes=[fod[q]])
                P.store("sp", "featc", featc[jj], fo[q][:], fod[q])
            c.barrier()
        if not FUSED:
            c.finish()
            return nc

    assert stage in ("rest", "all")
    if not FUSED:
        xT = P.din("xT", [128, NCH, EXT], F32)
        QT = P.din("QT", [16, 128, T], BF16)
        KTa = P.din("KTa", [4, 128, 8448], BF16)
        Va = P.din("Va", [4, 128, 66, 128], BF16)
        featc = P.din("featc", [16, 128, T], BF16)
    lnp = P.din("lnp", [4, 128, 2, NCH], F32)
    P.dram.update({"lnp%d" % i: lnp[i] for i in range(4)})
    for i in range(4):
        P.ddeps["lnp%d" % i] = P.ddeps["lnp"]
    w_out = [P.din("w_out%d" % l, [32, 128, NCH, 128], F32) for l in range(2)]
    wq = [P.din("wq%d" % l, [16, 128, NCH, 128], F32) for l in range(2)]
    k1T = [P.din("k1T%d" % l, [128, 8, 128], F32R) for l in range(2)]
    k2T = [P.din("k2T%d" % l, [128, 8, 128], F32R) for l in range(2)]
    UT = [P.din("UT%d" % l, [128, 128, NCH, 128], F32) for l in range(2)]
    VR = [P.din("VR%d" % l, [8, 128, 128, 512], F32) for l in range(2)]
    w1u = P.din("w1u", [32, 128, NCH, 128], F32)
    w1v = P.din("w1v", [8, 128, NCH, 512], F32)
    sgu_gb = P.din("sgu_gb", [2, D], F32)
    sguWT = P.din("sguWT", [128, 16, 128], F32)
    sgub2 = P.din("sgub2", [1, 32 * 128], F32)
    outT = P.dout("outT", [NCH, 128, T], F32)
    featA = P.dint("featA", [16, 128, T], BF16)
    uS = P.dint("uS", [NCH, 128, T], F32R)
    x1T = P.dint("x1T", [NCH, 128, T], F32)
    x2T = P.dint("x2T", [NCH, 128, T], F32)
    x3T = P.dint("x3T", [NCH, 128, T], F32)
    actT = P.dint("actT", [128, 128, T], BF16)
    WTd = P.dint("WTd", [128, 128, T], BF16)
    uT1 = P.dint("uT1", [NCH, 128, T], BF16)
    vtok = P.dint("vtok", [8, 128, D], F32)
    featB = P.dint("featB", [NCH, 128, T], BF16)

    class Resid:
        def __init__(self, ph, pfx, l, gmod, xname, xfn):
            self.l, self.gmod, self.xname, self.xfn = l, gmod, xname, xfn
            self.xs = [c.sbuf(pfx + "rx", [128, T], F32, ph) for _ in range(2)]
            self.xsd = deps(2)
            self.yg = [c.sbuf(pfx + "ryg", [128, 512], F32, ph) for _ in range(2)]
            self.ygd = deps(2)
            self.ut = [c.sbuf(pfx + "rut", [128, T], F32R, ph) for _ in range(2)]
            self.utd = deps(2)
            self.n = 0
            self.m = 0

        def __call__(self, ct, half, ps, psd):
            i = self.n % 2
            sl = slice(half * 512, half * 512 + 512)
            if half == 0:
                P.load("sp", self.xs[i][:], self.xname, self.xfn(ct), self.xsd[i])
            j = self.m % 2
            self.m += 1
            gcol = self.gmod * NCH + ct
            c.op("act", lambda e: e.activation(out=self.yg[j][:], in_=ps[:, :512], func=AF.Identity, scale=mod_sb[:, self.l, 0, gcol:gcol + 1]),
                 reads=[psd, P.d_mod], writes=[self.ygd[j]])
            c.op("dve", lambda e: e.scalar_tensor_tensor(out=self.ut[i][:, sl], in0=self.xs[i][:, sl], scalar=ALPHA, in1=self.yg[j][:],
                                                         op0=ALU.mult, op1=ALU.add), reads=[self.xsd[i], self.ygd[j]], pwrites=[self.utd[i]])
            if half == 1:
                P.store("sp", "uS", uS[ct], self.ut[i][:], self.utd[i])
                self.n += 1

    with ExitStack() as ph:
        kt = [c.sbuf("kt", [128, 8448], BF16, ph) for _ in range(2)]; ktd = deps(2)
        vt = [c.sbuf("vt", [128, 66, 128], BF16, ph) for _ in range(2)]; vtd = deps(2)
        qt = [c.sbuf("qt", [128, T], BF16, ph) for _ in range(2)]; qtd = deps(2)
        pt = [c.sbuf("pt", [128, 512], BF16, ph) for _ in range(4)]; ptd = deps(4)
        onb = c.sbuf("onb", [128, 128], BF16, ph); d_onb = Dep()
        c.op("dve", lambda e: e.memset(onb[:], 1.0), writes=[d_onb])
        ob = [c.sbuf("ob", [128, T], BF16, ph) for _ in range(2)]; obd = deps(2)
        rd = [c.sbuf("rd", [128, 512], F32, ph) for _ in range(2)]; rdd = deps(2)
        pS = [c.psum("pS", [128, 512], F32, ph) for _ in range(3)]; pSd = deps(3)
        pO = [c.psum("pO", [128, 512], F32, ph) for _ in range(2)]; pOd = deps(2)
        pD = [c.psum("pD", [128, 512], F32, ph) for _ in range(2)]; pDd = deps(2)
        SCALE = 128.0 ** -0.5
        ns = 0
        for h in range(16):
            g = h // 4
            gs = g % 2
            if h % 4 == 0:
                P.load("sp", kt[gs][:], "KTa", KTa[g], ktd[gs])
                P.load("sp", vt[gs][:], "Va", Va[g], vtd[gs])
            qi = h % 2
            P.load("sp", qt[qi][:], "QT", QT[h], qtd[qi])
            for half in range(2):
                ab = (h * 2 + half) % 2
                hsl = slice(half * 512, half * 512 + 512)
                def s_and_exp(st):
                    sb = (ns + st) % 3
                    pi = (ns + st) % 4
                    c.op("pe", lambda e: e.matmul(pS[sb][:], kt[gs][:, st * 128:(st + 1) * 128], qt[qi][:, hsl], start=True, stop=True),
                         reads=[ktd[gs], qtd[qi]], writes=[pSd[sb]])
                    c.op("act", lambda e: e.activation(out=pt[pi][:], in_=pS[sb][:], func=AF.Exp, scale=SCALE), reads=[pSd[sb]], writes=[ptd[pi]])

                def pv_den(st):
                    pi = (ns + st) % 4
                    c.op("pe", lambda e: e.matmul(pO[ab][:], vt[gs][:, st, :], pt[pi][:], start=(st == 0), stop=(st == 65)),
                         reads=[vtd[gs], ptd[pi]], writes=[pOd[ab]], inc=False)
                    c.op("pe", lambda e: e.matmul(pD[ab][:], onb[:], pt[pi][:], start=(st == 0), stop=(st == 65)),
                         reads=[d_onb, ptd[pi]], writes=[pDd[ab]], inc=True)
                LOOK = 2
                for st in range(min(LOOK, 66)):
                    s_and_exp(st)
                for st in range(66):
                    if st + LOOK < 66:
                        s_and_exp(st + LOOK)
                    pv_den(st)
                ns += 66
                ri = (h * 2 + half) % 2
                c.op("dve", lambda e: e.reciprocal(out=rd[ri][:], in_=pD[ab][:]), reads=[pDd[ab]], writes=[rdd[ri]])
                c.op("dve", lambda e: e.tensor_tensor(out=ob[qi][:, hsl], in0=pO[ab][:], in1=rd[ri][:], op=ALU.mult), reads=[pOd[ab], rdd[ri]], pwrites=[obd[qi]])
            P.store("sp", "featA", featA[h], ob[qi][:], obd[qi])
        c.barrier()

    def load_feat(ft, d0, d1, nameA, apA, nameB, apB):
        for ch in range(16):
            P.load("sp", ft[:, ch, :], nameA, apA[ch], d0, partial=True)
        for ch in range(16):
            P.load("sp", ft[:, 16 + ch, :], nameB, apB[ch], d1, partial=True)

    def wout_phase(pfx, l, feat_loader, xname, xfn):
        with ExitStack() as ph:
            ft = c.sbuf(pfx + "ft", [128, NCH, T], BF16, ph)
            d0, d1 = deps(2)
            feat_loader(ft, d0, d1)
            rs = Resid(ph, pfx, l, 2, xname, xfn)
            gemm_fm(P, ph, pfx + "g", "w_out%d" % l, w_out[l], 32, ft, [d0] * 16 + [d1] * 16, [(0, 512), (512, 512)],
                    lambda ct, j, ps, psd, n: rs(ct, j, ps, psd))
            c.barrier()

    def peer(l, hT, hTd, hstack, qstack, qT, xname, xd):
        pfx = "pr%d" % l
        with ExitStack() as ph:
            ao = [c.sbuf(pfx + "ao", [128, T], BF16, ph) for _ in range(2)]; aod = deps(2)

            def epi(ct, j, ps, psd, n):
                i = ct % 2
                c.op("act", lambda e: e.activation(out=ao[i][:, j * 512:(j + 1) * 512], in_=ps[:, :512], func=GELU), reads=[psd], pwrites=[aod[i]])
                if j == 1:
                    P.store("sp", "actT", actT[ct], ao[i][:], aod[i])
            gemm_fm(P, ph, pfx + "a1", "UT%d" % l, UT[l], 128, hT, hTd, [(0, 512), (512, 512)], epi)
            c.barrier()
        qTd = deps(16)
        with ExitStack() as ph:
            def epi(ct, j, ps, psd, n):
                c.op("act", lambda e: e.copy(qT[:, ct, j * 512:(j + 1) * 512], ps[:, :512]), reads=[psd], pwrites=[qTd[ct]])
            gemm_fm(P, ph, pfx + "wq", "wq%d" % l, wq[l], 16, hT, hTd, [(0, 512), (512, 512)], epi)
            c.barrier()
        hstack.close()
        S2a = c.sbuf(pfx + "S2a", [128, 8, T], F32R, qstack); d_S2a = deps(8)
        S2b = c.sbuf(pfx + "S2b", [128, 8, T], F32R, qstack); d_S2b = deps(8)
        k1s = c.sbuf(pfx + "k1", [128, 8, 128], F32R, qstack); d_k1 = Dep()
        k2s = c.sbuf(pfx + "k2", [128, 8, 128], F32R, qstack); d_k2 = Dep()
        idr = c.sbuf(pfx + "idr", [128, 128], F32R, qstack); d_idr = Dep()
        P.load("sp", k1s[:], "k1T%d" % l, k1T[l], d_k1)
        P.load("sp", k2s[:], "k2T%d" % l, k2T[l], d_k2)
        P.load("sp", idr[:], "ident_r", ident_r, d_idr)
        with ExitStack() as ph:
            S2 = c.sbuf(pfx + "S2", [128, 8, T], F32, ph); d_S2 = deps(8)
            idf = c.sbuf(pfx + "idf", [128, 128], F32, ph); d_idf = Dep()
            P.load("sp", idf[:], "ident_r", ident_r.bitcast(F32), d_idf)
            ST = c.sbuf(pfx + "ST", [128, 8, 16], F32, ph); d_ST = deps(8)
            STT = c.sbuf(pfx + "STT", [16, T], F32R, ph); d_STT = Dep()
            pA = [c.psum(pfx + "pA", [128, 512], F32, ph) for _ in range(2)]; pAd = deps(2)
            pB = [c.psum(pfx + "pB", [128, 512], F32, ph) for _ in range(2)]; pBd = deps(2)
            n = 0
            for h in range(8):
                for half in range(2):
                    b = n % 2; n += 1
                    sl = slice(half * 512, half * 512 + 512)
                    c.op("pe", lambda e: e.matmul(pA[b][:], k2s[:, h, :], qT[:, 2 * h + 1, sl], start=True, stop=True), reads=[d_k2, qTd[2 * h + 1]], writes=[pAd[b]])
                    c.op("act", lambda e: e.copy(S2[:, h, sl], pA[b][:]), reads=[pAd[b]], pwrites=[d_S2[h]])
            sc = [c.sbuf(pfx + "sc", [128, 256], F32, ph) for _ in range(2)]; scd = deps(2)
            tm = [c.sbuf(pfx + "tm", [128, 256], F32, ph) for _ in range(2)]; tmd = deps(2)
            tm2 = [c.sbuf(pfx + "tm2", [128, 256], F32, ph) for _ in range(2)]; tm2d = deps(2)
            vv = [c.sbuf(pfx + "vv", [128, 32], F32, ph) for _ in range(2)]; vvd = deps(2)
            cd = [c.sbuf(pfx + "cd", [128, 16, 16], F32, ph) for _ in range(2)]; cdd = deps(2)
            mm = [c.sbuf(pfx + "mm", [128, 24], F32, ph) for _ in range(2)]; mmd = deps(2)
            sm = [c.sbuf(pfx + "sm", [128, 8], F32, ph) for _ in range(2)]; smd = deps(2)
            ex = [c.sbuf(pfx + "ex", [128, 16], F32, ph) for _ in range(2)]; exd = deps(2)
            def chain(tt, h, b):
                tsl = slice(tt * 128, tt * 128 + 128)
                c.op("pe", lambda e: e.matmul(pB[b][:, 0:128], qT[:, 2 * h, tsl], k1s[:, h, :], start=True, stop=True), reads=[qTd[2 * h], d_k1], writes=[pBd[b]])
                yield
                c.op("pe", lambda e: e.matmul(pB[b][:, 128:256], qT[:, 2 * h + 1, tsl], k2s[:, h, :], start=True, stop=True), reads=[qTd[2 * h + 1], d_k2], writes=[pBd[b]])
                yield
                c.op("act", lambda e: e.copy(sc[b][:], pB[b][:, 0:256]), reads=[pBd[b]], writes=[scd[b]])
                yield
                for w_ in range(2):
                    o = w_ * 128
                    c.op("dve", lambda e: e.max(out=vv[b][:, w_ * 16:w_ * 16 + 8], in_=sc[b][:, o:o + 128]), reads=[scd[b]], pwrites=[vvd[b]])
                    yield
                    c.op("dve", lambda e: e.match_replace(out=tm[b][:, o:o + 128], in_to_replace=vv[b][:, w_ * 16:w_ * 16 + 8], in_values=sc[b][:, o:o + 128], imm_value=NEG),
                         reads=[scd[b], vvd[b]], pwrites=[tmd[b]])
                    yield
                    c.op("dve", lambda e: e.max(out=vv[b][:, w_ * 16 + 8:w_ * 16 + 16], in_=tm[b][:, o:o + 128]), reads=[tmd[b]], pwrites=[vvd[b]])
                    yield
                c.op("dve", lambda e: e.tensor_tensor(out=cd[b][:], in0=vv[b][:, 0:16].unsqueeze(2).to_broadcast([128, 16, 16]),
                                                      in1=vv[b][:, 16:32].unsqueeze(1).to_broadcast([128, 16, 16]), op=ALU.add), reads=[vvd[b]], writes=[cdd[b]])
                yield
                cflat = cd[b][:].rearrange("p a b -> p (a b)")
                c.op("dve", lambda e: e.max(out=mm[b][:, 0:8], in_=cflat), reads=[cdd[b]], pwrites=[mmd[b]])
                yield
                c.op("dve", lambda e: e.match_replace(out=tm[b][:], in_to_replace=mm[b][:, 0:8], in_values=cflat, imm_value=NEG), reads=[cdd[b], mmd[b]], writes=[tmd[b]])
                yield
                c.op("dve", lambda e: e.max(out=mm[b][:, 8:16], in_=tm[b][:]), reads=[tmd[b]], pwrites=[mmd[b]])
                yield
                c.op("dve", lambda e: e.match_replace(out=tm2[b][:], in_to_replace=mm[b][:, 8:16], in_values=tm[b][:], imm_value=NEG), reads=[tmd[b], mmd[b]], writes=[tm2d[b]])
                yield
                c.op("dve", lambda e: e.max(out=mm[b][:, 16:24], in_=tm2[b][:]), reads=[tm2d[b]], pwrites=[mmd[b]])
                yield
                c.op("dve", lambda e: e.tensor_tensor(out=sm[b][:, 0:1], in0=mm[b][:, 15:16], in1=mm[b][:, 16:17], op=ALU.add), reads=[mmd[b]], pwrites=[smd[b]])
                yield
                c.op("dve", lambda e: e.tensor_scalar(out=ST[:, tt, h:h + 1], in0=sm[b][:, 0:1], scalar1=-0.5, scalar2=None, op0=ALU.mult), reads=[smd[b]], pwrites=[d_ST[tt]])
                yield
                c.op("dve", lambda e: e.tensor_scalar(out=sm[b][:, 1:2], in0=mm[b][:, 0:1], scalar1=-1.0, scalar2=None, op0=ALU.mult), reads=[mmd[b]], pwrites=[smd[b]])
                yield
                c.op("act", lambda e: e.activation(out=ex[b][:], in_=mm[b][:, 0:16], func=AF.Exp, bias=sm[b][:, 1:2], scale=1.0, accum_out=sm[b][:, 2:3]),
                     reads=[mmd[b], smd[b]], writes=[exd[b]], pwrites=[smd[b]])
                yield
                c.op("act", lambda e: e.activation(out=sm[b][:, 3:4], in_=sm[b][:, 2:3], func=AF.Ln), reads=[smd[b], exd[b]], pwrites=[smd[b]])
                yield
                c.op("dve", lambda e: e.tensor_tensor(out=ST[:, tt, 8 + h:9 + h], in0=sm[b][:, 1:2], in1=sm[b][:, 3:4], op=ALU.subtract), reads=[smd[b]], pwrites=[d_ST[tt]])
                yield
            its = [(tt, h) for tt in range(8) for h in range(8)]
            for q_ in range(0, 64, 2):
                gens = [chain(its[q_][0], its[q_][1], 0), chain(its[q_ + 1][0], its[q_ + 1][1], 1)]
                live = list(gens)
                while live:
                    for g_ in list(live):
                        try:
                            next(g_)
                        except StopIteration:
                            live.remove(g_)
            for tt in range(8):
                b = tt % 2
                c.op("pe", lambda e: e.transpose(out=pA[b][0:16, 0:128], in_=ST[:, tt, :], identity=idf[:]), reads=[d_ST[tt], d_idf], writes=[pAd[b]])
                c.op("dve", lambda e: e.tensor_copy(out=STT[:, tt * 128:(tt + 1) * 128], in_=pA[b][0:16, 0:128]), reads=[pAd[b]], pwrites=[d_STT])
            n = 0
            for h in range(8):
                for half in range(2):
                    sl = slice(half * 512, half * 512 + 512)
                    for which, dst, dd in ((0, S2a, d_S2a), (1, S2b, d_S2b)):
                        b = n % 2; n += 1
                        r_ = which * 8 + h
                        c.op("pe", lambda e: e.matmul(pA[b][:], idr[0:16, r_:r_ + 1].to_broadcast([16, 128]), STT[:, sl], start=True, stop=True),
                             reads=[d_idr, d_STT], writes=[pAd[b]])
                        c.op("dve", lambda e: e.tensor_tensor(out=dst[:, h, sl], in0=pA[b][:], in1=S2[:, h, sl], op=ALU.add), reads=[pAd[b], d_S2[h]], pwrites=[dd[h]])
            c.barrier()
        with ExitStack() as ph:
            at = [c.sbuf(pfx + "at", [128, T], BF16, ph) for _ in range(2)]; atd = deps(2)
            pe_ = [c.sbuf(pfx + "pe", [128, 512], F32, ph) for _ in range(3)]; ped = deps(3)
            tp = [c.sbuf(pfx + "tp", [128, 512], F32, ph) for _ in range(3)]; tpd = deps(3)
            acc = [c.sbuf(pfx + "acc", [128, T], F32, ph) for _ in range(2)]; accd = [deps(2), deps(2)]
            wo = [c.sbuf(pfx + "wo", [128, T], BF16, ph) for _ in range(2)]; wod = deps(2)
            pA = [c.psum(pfx + "qA", [128, 512], F32, ph) for _ in range(4)]; pAd = deps(4)
            pB = [c.psum(pfx + "qB", [128, 512], F32, ph) for _ in range(4)]; pBd = deps(4)
            n = 0
            for i in range(128):
                ai = i % 2
                P.load("sp", at[ai][:], "actT", actT[i], atd[ai])
                for h in range(8):
                    for half in range(2):
                        b = n % 4
                        t3 = n % 3
                        n += 1
                        sl = slice(half * 512, half * 512 + 512)
                        lhs = k1s[:, h, i:i + 1].to_broadcast([128, 128])
                        c.op("pe", lambda e: e.matmul(pA[b][:], lhs, qT[:, 2 * h, sl], start=True, stop=False), reads=[d_k1, qTd[2 * h]], writes=[pAd[b]], inc=False)
                        c.op("pe", lambda e: e.matmul(pA[b][:], idr[:], S2a[:, h, sl], start=False, stop=True), reads=[d_idr, d_S2a[h]], writes=[pAd[b]], inc=True)
                        c.op("pe", lambda e: e.matmul(pB[b][:], lhs, qT[:, 2 * h, sl], start=True, stop=False), reads=[d_k1, qTd[2 * h]], writes=[pBd[b]], inc=False)
                        c.op("pe", lambda e: e.matmul(pB[b][:], idr[:], S2b[:, h, sl], start=False, stop=True), reads=[d_idr, d_S2b[h]], writes=[pBd[b]], inc=True)
                        c.op("act", lambda e: e.activation(out=pe_[t3][:], in_=pB[b][:], func=AF.Exp), reads=[pBd[b]], writes=[ped[t3]])
                        if h == 0:
                            c.op("dve", lambda e: e.scalar_tensor_tensor(out=acc[ai][:, sl], in0=pA[b][:], scalar=0.0, in1=pe_[t3][:], op0=ALU.is_ge, op1=ALU.mult),
                                 reads=[pAd[b], ped[t3]], writes=[accd[ai][half]])
                        else:
                            c.op("dve", lambda e: e.scalar_tensor_tensor(out=tp[t3][:], in0=pA[b][:], scalar=0.0, in1=pe_[t3][:], op0=ALU.is_ge, op1=ALU.mult),
                                 reads=[pAd[b], ped[t3]], writes=[tpd[t3]])
                            c.op("pool", lambda e: e.tensor_tensor(out=acc[ai][:, sl], in0=acc[ai][:, sl], in1=tp[t3][:], op=ALU.add),
                                 reads=[tpd[t3], accd[ai][half]], writes=[accd[ai][half]])
                c.op("dve", lambda e: e.tensor_tensor(out=wo[ai][:], in0=acc[ai][:], in1=at[ai][:], op=ALU.mult), reads=[accd[ai][0], accd[ai][1], atd[ai]], writes=[wod[ai]])
                P.store("sp", "WTd", WTd[i], wo[ai][:], wod[ai])
            c.barrier()
        qstack.close()
        with ExitStack() as ph:
            rs = Resid(ph, pfx + "b", l, 5, xname, lambda ct: xd[ct])
            wt = [c.sbuf(pfx + "wt", [128, T], BF16, ph) for _ in range(8)]; wtd = deps(8)
            vs_ = [c.sbuf(pfx + "vs", [128, 512], BF16, ph) for _ in range(8)]; vsd = deps(8)
            pp = [c.psum(pfx + "pp", [128, 512], F32, ph) for _ in range(8)]; ppd = deps(8)
            n = 0
            for p_ in range(8):
                for et in range(128):
                    s = n % 8; n += 1
                    P.load("sp", wt[s][:], "WTd", WTd[et], wtd[s])
                    P.load("pool", vs_[s][:], "VR%d" % l, VR[l][p_, et], vsd[s], **cast_dma_kw())
                    for dc in range(4):
                        for half in range(2):
                            b = dc * 2 + half
                            c.op("pe", lambda e: e.matmul(pp[b][:], vs_[s][:, dc * 128:(dc + 1) * 128], wt[s][:, half * 512:(half + 1) * 512],
                                                          start=(et == 0), stop=(et == 127)),
                                 reads=[vsd[s], wtd[s]], writes=[ppd[b]], inc=(et == 127 or (dc == 3 and half == 1)))
                for dc in range(4):
                    for half in range(2):
                        rs(p_ * 4 + dc, half, pp[dc * 2 + half], ppd[dc * 2 + half])
            c.barrier()

    wout_phase("wo0", 0, lambda ft, d0, d1: load_feat(ft, d0, d1, "featA", featA, "featc", featc), "xT", lambda ct: xT[:, ct, 16:16 + T])
    qs = ExitStack()
    qT = c.sbuf("qT_a", [128, 16, T], F32R, qs)
    hs = ExitStack()
    hT = c.sbuf("hT_a", [128, NCH, T], BF16, hs); hTd = deps(NCH)
    ln_phase(P, "ln0", "uS", "lnp0", "x1T", hT, hTd, mod_sb, 0, 3, 4)
    peer(0, hT, hTd, hs, qs, qT, "x1T", x1T)
    hs = ExitStack()
    hT = c.sbuf("hT_b", [128, NCH, T], BF16, hs); hTd = deps(NCH)
    ln_phase(P, "ln1", "uS", "lnp1", "x2T", hT, hTd, mod_sb, 1, 0, 1)
    with ExitStack() as ph:
        uo = [c.sbuf("g1uo", [128, T], BF16, ph) for _ in range(2)]; uod = deps(2)

        def epi(ct, j, ps, psd, n):
            i = ct % 2
            c.op("act", lambda e: e.activation(out=uo[i][:, j * 512:(j + 1) * 512], in_=ps[:, :512], func=GELU), reads=[psd], pwrites=[uod[i]])
            if j == 1:
                P.store("sp", "uT1", uT1[ct], uo[i][:], uod[i])
        gemm_fm(P, ph, "g1u", "w1u", w1u, 32, hT, hTd, [(0, 512), (512, 512)], epi)
        c.barrier()
    with ExitStack() as ph:
        wv_ = [c.sbuf("g1wv", [128, NCH, 512], BF16, ph) for _ in range(2)]; wvd = deps(2)
        vo_ = [c.sbuf("g1vo", [128, 512], F32, ph) for _ in range(3)]; vod_ = deps(3)
        pv = [c.psum("g1pv", [128, 512], F32, ph) for _ in range(4)]; pvd = deps(4)
        n = 0
        for ct in range(8):
            s = ct % 2
            P.load("pool", wv_[s][:], "w1v", w1v[ct], wvd[s], **cast_dma_kw())
            for tt in range(8):
                b = n % 4; o3 = n % 3; n += 1
                for k in range(NCH):
                    c.op("pe", lambda e: e.matmul(pv[b][:], hT[:, k, tt * 128:(tt + 1) * 128], wv_[s][:, k, :], start=(k == 0), stop=(k == NCH - 1)),
                         reads=[hTd[k], wvd[s]], writes=[pvd[b]], inc=(k == NCH - 1))
                c.op("act", lambda e: e.activation(out=vo_[o3][:], in_=pv[b][:], func=GELU), reads=[pvd[b]], writes=[vod_[o3]])
                P.store("sp", "vtok", vtok[tt][:, ct * 512:(ct + 1) * 512], vo_[o3][:], vod_[o3])
        c.barrier()
    hs.close()
    with ExitStack() as ph:
        uts = c.sbuf("g2u", [128, NCH, T], BF16, ph); d_uts = Dep()
        for ch in range(NCH):
            P.load("sp", uts[:, ch, :], "uT1", uT1[ch], d_uts, partial=True)
        gbc = c.sbuf("g2g", [128, 2, D], F32, ph); d_gbc = Dep()
        P.load("sp", gbc[:, 0, :], "sgu_gb", sgu_gb[0:1, :].partition_broadcast(128), d_gbc, partial=True)
        P.load("sp", gbc[:, 1, :], "sgu_gb", sgu_gb[1:2, :].partition_broadcast(128), d_gbc, partial=True)
        swt = c.sbuf("g2w", [128, 16, 128], BF16, ph); d_swt = Dep()
        P.load("pool", swt[:], "sguWT", sguWT, d_swt, **cast_dma_kw())
        sb2 = c.sbuf("g2b", [128, 32 * 128], F32, ph); d_sb2 = Dep()
        P.load("sp", sb2[:], "sgub2", sgub2.partition_broadcast(128), d_sb2)
        vt_ = [c.sbuf("g2v", [128, D], F32, ph) for _ in range(2)]; vtd_ = deps(2)
        vln = [c.sbuf("g2vl", [128, D], BF16, ph) for _ in range(2)]; vlnd = deps(2)
        bst = [c.sbuf("g2bs", [128, 8, 6], F32, ph) for _ in range(2)]; bstd = deps(2)
        mv = [c.sbuf("g2mv", [128, 4], F32, ph) for _ in range(2)]; mvd = deps(2)
        tmpb = [c.sbuf("g2t", [128, 512], F32, ph) for _ in range(2)]; tmpd = deps(2)
        og = [c.sbuf("g2o", [128, 4, 128], BF16, ph) for _ in range(3)]; ogd = deps(3)
        pm = [c.psum("g2p", [128, 512], F32, ph) for _ in range(4)]; pmd = deps(4)
        n = 0
        for tt in range(8):
            s = tt % 2
            P.load("sp", vt_[s][:], "vtok", vtok[tt], vtd_[s])
            for j in range(8):
                c.op("dve", lambda e: e.bn_stats(out=bst[s][:, j, :], in_=vt_[s][:, j * 512:(j + 1) * 512]), reads=[vtd_[s]], pwrites=[bstd[s]])
            c.op("dve", lambda e: e.bn_aggr(out=mv[s][:, 0:2], in_=bst[s][:].rearrange("p a b -> p (a b)")), reads=[bstd[s]], pwrites=[mvd[s]])
            c.op("dve", lambda e: e.tensor_scalar(out=mv[s][:, 2:3], in0=mv[s][:, 1:2], scalar1=EPS, scalar2=None, op0=ALU.add), reads=[mvd[s]], pwrites=[mvd[s]])
            c.op("act", lambda e: e.activation(out=mv[s][:, 2:3], in_=mv[s][:, 2:3], func=AF.Sqrt), reads=[mvd[s]], pwrites=[mvd[s]])
            c.op("dve", lambda e: e.reciprocal(out=mv[s][:, 3:4], in_=mv[s][:, 2:3]), reads=[mvd[s]], pwrites=[mvd[s]])
            c.op("dve", lambda e: e.tensor_scalar(out=vt_[s][:], in0=vt_[s][:], scalar1=mv[s][:, 0:1], scalar2=mv[s][:, 3:4], op0=ALU.subtract, op1=ALU.mult),
                 reads=[mvd[s]], writes=[vtd_[s]])
            c.op("pool", lambda e: e.tensor_tensor(out=vt_[s][:], in0=vt_[s][:], in1=gbc[:, 0, :], op=ALU.mult), reads=[d_gbc], writes=[vtd_[s]])
            c.op("dve", lambda e: e.tensor_tensor(out=vln[s][:], in0=vt_[s][:], in1=gbc[:, 1, :], op=ALU.add), reads=[vtd_[s], d_gbc], writes=[vlnd[s]])
            for cg in range(8):
                b = n % 4; t2_ = n % 2; o3 = n % 3; n += 1
                for cc in range(4):
                    ch = cg * 4 + cc
                    c.op("pe", lambda e: e.matmul(pm[b][:, cc * 128:(cc + 1) * 128], vln[s][:, ch * 128:(ch + 1) * 128], swt[:, ch // 2, :], start=True, stop=True),
                         reads=[vlnd[s], d_swt], writes=[pmd[b]], inc=(cc == 3))
                c.op("dve", lambda e: e.tensor_tensor(out=tmpb[t2_][:], in0=pm[b][:], in1=sb2[:, cg * 512:(cg + 1) * 512], op=ALU.add), reads=[pmd[b], d_sb2], writes=[tmpd[t2_]])
                c.op("dve", lambda e: e.tensor_tensor(out=og[o3][:], in0=tmpb[t2_][:].rearrange("p (a b) -> p a b", a=4), in1=uts[:, cg * 4:(cg + 1) * 4, tt * 128:(tt + 1) * 128], op=ALU.mult),
                     reads=[tmpd[t2_], d_uts], writes=[ogd[o3]])
                P.store("sp", "featB", featB[cg * 4:(cg + 1) * 4, :, tt * 128:(tt + 1) * 128].rearrange("c p t -> p c t"), og[o3][:], ogd[o3])
        c.barrier()

    def load_featB(ft, d0, d1):
        for ch in range(16):
            P.load("sp", ft[:, ch, :], "featB", featB[ch], d0, partial=True)
        for ch in range(16, 32):
            P.load("sp", ft[:, ch, :], "featB", featB[ch], d1, partial=True)
    wout_phase("wo1", 1, load_featB, "x2T", lambda ct: x2T[ct])
    qs = ExitStack()
    qT = c.sbuf("qT_b", [128, 16, T], F32R, qs)
    hs = ExitStack()
    hT = c.sbuf("hT_c", [128, NCH, T], BF16, hs); hTd = deps(NCH)
    ln_phase(P, "ln2", "uS", "lnp2", "x3T", hT, hTd, mod_sb, 1, 3, 4)
    peer(1, hT, hTd, hs, qs, qT, "x3T", x3T)
    ln_phase(P, "ln3", "uS", "lnp3", "outT")
    c.finish()
    return nc


def fm_vec(v):
    v = np.asarray(v, np.float32).reshape(-1, 128)
    return np.ascontiguousarray(v.T)


def wtiles(w):
    K, N = w.shape
    return np.ascontiguousarray(w.reshape(K // 128, 128, N // 128, 128).transpose(2, 1, 0, 3))


def run(stage, in_maps):
    nc = build(stage)
    res = run_bass_kernel_spmd(nc, in_maps, core_ids=list(range(NCORES)))
    return res.results


def rope_tables():
    n_freq = 32
    inv = (10000.0 ** (-np.arange(n_freq, dtype=np.float32) / n_freq)).astype(np.float32)
    t = np.arange(8192)
    rows = (t // 64).astype(np.float32)
    cols = (t % 64).astype(np.float32)
    ang_r = rows[:, None] * inv[None, :]
    ang_c = cols[:, None] * inv[None, :]
    cosd = np.concatenate([np.cos(ang_r), np.cos(ang_r), np.cos(ang_c), np.cos(ang_c)], axis=1).astype(np.float32)
    sind = np.concatenate([np.sin(ang_r), np.sin(ang_r), np.sin(ang_c), np.sin(ang_c)], axis=1).astype(np.float32)
    return cosd, sind


def rot_matrix():
    Rm = np.zeros((128, 128), np.float32)
    for blk in (0, 64):
        for d in range(32):
            Rm[blk + d + 32, blk + d] = -1.0
            Rm[blk + d, blk + d + 32] = 1.0
    return Rm


def stage_mod(inp):
    cc = np.stack([fm_vec(inp["c"][0]), fm_vec(inp["c_ctx"])], axis=-1)
    maps = []
    for r in range(NCORES):
        m = {"cc": cc}
        bm = np.zeros((2, 2, 3072), np.float32)
        for l in range(2):
            w = inp["l%d_w_mod" % l][:, r * 3072:(r + 1) * 3072]
            m["wm%d" % l] = np.ascontiguousarray(w.reshape(NCH, 128, 3072))
            bm[l, :, :] = inp["l%d_b_mod" % l][r * 3072:(r + 1) * 3072][None, :]
        m["bm"] = bm
        maps.append(m)
    res = run("mod", maps)
    full = np.concatenate([res[r]["modout"] for r in range(NCORES)], axis=-1)
    modv = np.ascontiguousarray(full.reshape(2, 2, 192, 128).transpose(3, 0, 1, 2))
    return modv


def consts():
    return {"ident_r": np.eye(128, dtype=np.float32), "ones_r": np.ones((128, 128), np.float32)}


def stage_l0a(inp, modv):
    x = inp["x"][0]
    xpad = np.zeros((8192 + 32, D), np.float32)
    xpad[16:16 + 8192] = x
    ctxT = np.ascontiguousarray(inp["ctx"][0].T.reshape(NCH, 128, 256).transpose(1, 0, 2))
    w = inp["l0_w_in"]
    order = list(range(0, 24 * 128))
    wt = wtiles(w)
    perm = list(range(24))
    for j in range(16):
        perm += [24 + j, 40 + j]
    wt = np.ascontiguousarray(wt[perm])
    gains = np.stack([inp["l0_q_gain"], inp["l0_k_gain"]], axis=1).astype(np.float32)
    cosd, sind = rope_tables()
    convp = np.zeros((128, 16, 34), np.float32)
    convp[:, :, 0:31] = inp["l0_dw_w"].T.reshape(16, 128, 31).transpose(1, 0, 2)
    convp[:, :, 31] = inp["l0_dw_b"].reshape(16, 128).T
    convp[:, :, 32] = inp["l0_conv_ln_g"].reshape(16, 128).T
    convp[:, :, 33] = inp["l0_conv_ln_b"].reshape(16, 128).T
    Rm = rot_matrix()
    maps = []
    for r in range(NCORES):
        xe = xpad[r * T:r * T + EXT]
        xT = np.ascontiguousarray(xe.T.reshape(NCH, 128, EXT).transpose(1, 0, 2))
        hm = np.ones((1, EXT), np.float32)
        gt = r * T - 16 + np.arange(EXT)
        hm[0, (gt < 0) | (gt >= 8192)] = 0.0
        m = dict(consts())
        m.update({"modv": modv, "xT": xT, "hmask": hm, "ctxT": ctxT, "w_in0": wt, "gains": gains,
                  "cosT": np.ascontiguousarray(cosd[r * T:(r + 1) * T].T), "sinT": np.ascontiguousarray(sind[r * T:(r + 1) * T].T),
                  "Rm": Rm, "convp": convp})
        maps.append(m)
    return run("l0a", maps), maps


def stage_rest(inp, modv, res_a, maps_a):
    KTc = np.asarray(res_a[0]["KTc"])
    Vc = np.asarray(res_a[0]["Vc"])
    KTa = np.concatenate([KTc] + [np.asarray(res_a[r]["KTl"]) for r in range(NCORES)], axis=2)
    Vall = np.concatenate([Vc] + [np.asarray(res_a[r]["Vl"]) for r in range(NCORES)], axis=1)
    Va = np.ascontiguousarray(Vall.reshape(4, 66, 128, 128).transpose(0, 2, 1, 3))
    lnp = np.stack([np.stack([fm_vec(inp[p + "_ln_g"]), fm_vec(inp[p + "_ln_b"])], axis=1)
                    for p in ("l0_mix", "l0_ffn", "l1_mix", "l1_ffn")], axis=0)
    shared = dict(consts())
    shared.update({"modv": modv, "KTa": KTa, "Va": Va, "lnp": np.ascontiguousarray(lnp)})
    for l in range(2):
        p = "l%d_" % l
        shared["w_out%d" % l] = wtiles(inp[p + "w_out"])
        shared["wq%d" % l] = wtiles(inp[p + "peer_wq"])
        shared["k1T%d" % l] = np.ascontiguousarray(inp[p + "peer_k1"].transpose(2, 0, 1))
        shared["k2T%d" % l] = np.ascontiguousarray(inp[p + "peer_k2"].transpose(2, 0, 1))
        shared["UT%d" % l] = wtiles(inp[p + "peer_u"].T)
        shared["VR%d" % l] = np.ascontiguousarray(inp[p + "peer_v"].reshape(128, 128, 8, 512).transpose(2, 0, 1, 3))
    w1 = inp["l1_w_in"]
    shared["w1u"] = wtiles(w1[:, :4096])
    shared["w1v"] = np.ascontiguousarray(w1[:, 4096:].reshape(NCH, 128, 8, 512).transpose(2, 1, 0, 3))
    shared["sgu_gb"] = np.stack([inp["l1_sgu_ln_g"], inp["l1_sgu_ln_b"]], axis=0).astype(np.float32)
    shared["sguWT"] = np.ascontiguousarray(inp["l1_sgu_w"].transpose(2, 0, 1))
    shared["sgub2"] = np.ascontiguousarray(np.repeat(inp["l1_sgu_b"], 2, axis=0).reshape(1, 32 * 128))
    maps = []
    for r in range(NCORES):
        m = dict(shared)
        m["xT"] = maps_a[r]["xT"]
        m["QT"] = np.asarray(res_a[r]["QT"])
        m["featc"] = np.asarray(res_a[r]["featc"])
        maps.append(m)
    res = run("rest", maps)
    out = np.empty((1, 8192, D), np.float32)
    for r in range(NCORES):
        o = np.asarray(res[r]["outT"])
        out[0, r * T:(r + 1) * T] = o.transpose(2, 0, 1).reshape(T, D)
    return out


def stage_all(inp):
    L = [
        dict(w_mod=inp["l0_w_mod"], b_mod=inp["l0_b_mod"], w_out=inp["l0_w_out"], mix_g=inp["l0_mix_ln_g"], mix_b=inp["l0_mix_ln_b"],
             wq=inp["l0_peer_wq"], k1=inp["l0_peer_k1"], k2=inp["l0_peer_k2"], u=inp["l0_peer_u"], v=inp["l0_peer_v"],
             ffn_g=inp["l0_ffn_ln_g"], ffn_b=inp["l0_ffn_ln_b"]),
        dict(w_mod=inp["l1_w_mod"], b_mod=inp["l1_b_mod"], w_out=inp["l1_w_out"], mix_g=inp["l1_mix_ln_g"], mix_b=inp["l1_mix_ln_b"],
             wq=inp["l1_peer_wq"], k1=inp["l1_peer_k1"], k2=inp["l1_peer_k2"], u=inp["l1_peer_u"], v=inp["l1_peer_v"],
             ffn_g=inp["l1_ffn_ln_g"], ffn_b=inp["l1_ffn_ln_b"]),
    ]
    x = inp["x"][0]
    sh = dict(consts())
    sh["cc"] = np.stack([fm_vec(inp["c"][0]), fm_vec(inp["c_ctx"])], axis=-1)
    sh["bmT"] = np.ascontiguousarray(np.stack([fm_vec(L[0]["b_mod"]), fm_vec(L[1]["b_mod"])], axis=1))
    lnp = []
    for l in range(2):
        sh["wmT%d" % l] = wtiles(L[l]["w_mod"])
        sh["w_out%d" % l] = wtiles(L[l]["w_out"])
        sh["wq%d" % l] = wtiles(L[l]["wq"])
        sh["k1T%d" % l] = np.ascontiguousarray(L[l]["k1"].transpose(2, 0, 1))
        sh["k2T%d" % l] = np.ascontiguousarray(L[l]["k2"].transpose(2, 0, 1))
        sh["UT%d" % l] = wtiles(L[l]["u"].T)
        sh["VR%d" % l] = np.ascontiguousarray(L[l]["v"].reshape(128, 128, 8, 512).transpose(2, 0, 1, 3))
        lnp.append(np.stack([fm_vec(L[l]["mix_g"]), fm_vec(L[l]["mix_b"])], axis=1))
        lnp.append(np.stack([fm_vec(L[l]["ffn_g"]), fm_vec(L[l]["ffn_b"])], axis=1))
    sh["lnp"] = np.ascontiguousarray(np.stack(lnp, axis=0))
    w0 = inp["l0_w_in"]
    wt = wtiles(w0)
    perm = list(range(24))
    for j in range(16):
        perm += [24 + j, 40 + j]
    sh["w_in0"] = np.ascontiguousarray(wt[perm])
    sh["w_v0"] = np.ascontiguousarray(w0[:, 2560:3072].reshape(NCH, 128, 512).transpose(1, 0, 2))
    sh["gains"] = np.stack([inp["l0_q_gain"], inp["l0_k_gain"]], axis=1).astype(np.float32)
    cosd, sind = rope_tables()
    sh["cosA"] = np.ascontiguousarray(cosd.T)
    sh["sinA"] = np.ascontiguousarray(sind.T)
    sh["Rm"] = rot_matrix()
    convp = np.zeros((128, 16, 34), np.float32)
    convp[:, :, 0:31] = inp["l0_dw_w"].T.reshape(16, 128, 31).transpose(1, 0, 2)
    convp[:, :, 31] = inp["l0_dw_b"].reshape(16, 128).T
    convp[:, :, 32] = inp["l0_conv_ln_g"].reshape(16, 128).T
    convp[:, :, 33] = inp["l0_conv_ln_b"].reshape(16, 128).T
    sh["convp"] = convp
    sh["ctxT"] = np.ascontiguousarray(inp["ctx"][0].T.reshape(NCH, 128, 256).transpose(1, 0, 2))
    sh["xTa"] = np.ascontiguousarray(x.T.reshape(NCH, 128, 8192).transpose(1, 0, 2))
    w1 = inp["l1_w_in"]
    sh["w1u"] = wtiles(w1[:, :4096])
    sh["w1v"] = np.ascontiguousarray(w1[:, 4096:].reshape(NCH, 128, 8, 512).transpose(2, 1, 0, 3))
    sh["sgu_gb"] = np.stack([inp["l1_sgu_ln_g"], inp["l1_sgu_ln_b"]], axis=0).astype(np.float32)
    sh["sguWT"] = np.ascontiguousarray(inp["l1_sgu_w"].transpose(2, 0, 1))
    sh["sgub2"] = np.ascontiguousarray(np.repeat(inp["l1_sgu_b"], 2, axis=0).reshape(1, 32 * 128))
    xpad = np.zeros((8192 + 32, D), np.float32)
    xpad[16:16 + 8192] = x
    maps = []
    for r in range(NCORES):
        m = dict(sh)
        xe = xpad[r * T:r * T + EXT]
        m["xT"] = np.ascontiguousarray(xe.T.reshape(NCH, 128, EXT).transpose(1, 0, 2))
        hm = np.ones((1, EXT), np.float32)
        gt = r * T - 16 + np.arange(EXT)
        hm[0, (gt < 0) | (gt >= 8192)] = 0.0
        m["hmask"] = hm
        m["cosT"] = np.ascontiguousarray(cosd[r * T:(r + 1) * T].T)
        m["sinT"] = np.ascontiguousarray(sind[r * T:(r + 1) * T].T)
        maps.append(m)
    res = run("all", maps)
    out = np.empty((1, 8192, D), np.float32)
    for r in range(NCORES):
        o = np.asarray(res[r]["outT"])
        out[0, r * T:(r + 1) * T] = o.transpose(2, 0, 1).reshape(T, D)
    return out


def kernel(**inp):
    inp = {k: np.asarray(v) for k, v in inp.items()}
    return stage_all(inp)
```
